# Optimizing a Trainium2 kernel written in Bass

```python
import math
import jax, jax.numpy as jnp
from jax import lax
import numpy as np

D_MODEL = 1024
BATCH = 32
SEQ = 2048
DEPTH = 1

HEAD_DIM = 64
DSA_HEADS = 8
KV_LATENT = 256
IDX_HEADS = 4
IDX_DIM = 64
TOPK_MAX = 256
Q_BLOCK = 128
DIL_GROUPS = ((128, 1), (512, 4), (2048, 16))
N_DIL_GROUPS = 3
DIL_HEADS_PER_GROUP = 4
DIL_HEADS = N_DIL_GROUPS * DIL_HEADS_PER_GROUP
N_BRANCHES = 2
NUM_BUCKETS = 32
MAX_DISTANCE = 2048
N_REL_HEADS = DSA_HEADS + DIL_HEADS
N_GROUPS = 4
EXPERTS_PER_GROUP = 8
N_EXPERTS = N_GROUPS * EXPERTS_PER_GROUP
EXPERT_FF = 512
TOP_K_INNER = 2
RMS_EPS = 1e-6
NEG = -1e30

COLS_DSA_Q = DSA_HEADS * HEAD_DIM
COLS_KV = KV_LATENT
COLS_IDX_Q = IDX_HEADS * IDX_DIM
COLS_IDX_K = IDX_DIM
COLS_IDX_W = IDX_HEADS
COLS_DIL = 3 * DIL_HEADS * HEAD_DIM
COLS_GATE = N_BRANCHES * D_MODEL
IN_COLS = COLS_DSA_Q + COLS_KV + COLS_IDX_Q + COLS_IDX_K + COLS_IDX_W + COLS_DIL + COLS_GATE
SPLIT_POINTS = [COLS_DSA_Q,
                COLS_DSA_Q + COLS_KV,
                COLS_DSA_Q + COLS_KV + COLS_IDX_Q,
                COLS_DSA_Q + COLS_KV + COLS_IDX_Q + COLS_IDX_K,
                COLS_DSA_Q + COLS_KV + COLS_IDX_Q + COLS_IDX_K + COLS_IDX_W,
                COLS_DSA_Q + COLS_KV + COLS_IDX_Q + COLS_IDX_K + COLS_IDX_W + COLS_DIL]
DIL_OUT = DIL_HEADS_PER_GROUP * HEAD_DIM

kernel_name = "hybrid_dsa_dilated_hmoe_block"


def rms_norm(x, g):
    xf = x.astype(jnp.float32)
    y = xf * lax.rsqrt(jnp.mean(xf * xf, axis=-1, keepdims=True) + RMS_EPS)
    return (y * g.astype(jnp.float32)).astype(x.dtype)


def rel_bucket(dist):
    max_exact = NUM_BUCKETS // 2
    n = jnp.maximum(dist, 0)
    nf = jnp.maximum(n, 1).astype(jnp.float32)
    large = max_exact + (jnp.log(nf / max_exact) / math.log(MAX_DISTANCE / max_exact)
                         * (NUM_BUCKETS - max_exact)).astype(jnp.int32)
    large = jnp.minimum(large, NUM_BUCKETS - 1)
    return jnp.where(n < max_exact, n, large)


def dsa_branch(q, c, q_idx, k_idx, w_idx, w_uk, w_uv, bias_tab):
    B, L = c.shape[:2]
    topk = min(TOPK_MAX, L // 4)
    nb = L // Q_BLOCK
    q_lat = jnp.einsum('blhd,hcd->blhc', q, w_uk) * HEAD_DIM ** -0.5
    k_idx_f = k_idx.astype(jnp.float32)
    bias_tab_f = bias_tab.astype(jnp.float32)
    key_pos = jnp.arange(L)

    def to_blocks(a):
        return jnp.moveaxis(a.reshape((B, nb, Q_BLOCK) + a.shape[2:]), 1, 0)

    def block_fn(args):
        n, qlb, qib, wib = args
        qpos = n * Q_BLOCK + jnp.arange(Q_BLOCK)
        s = jnp.einsum('bqhd,bsd->bqhs', qib.astype(jnp.float32), k_idx_f) * IDX_DIM ** -0.5
        score = jnp.einsum('bqh,bqhs->bqs', wib.astype(jnp.float32) * IDX_HEADS ** -0.5,
                           jax.nn.relu(s))
        causal = key_pos[None, :] <= qpos[:, None]
        score = jnp.where(causal[None], score, NEG)
        _, sel = lax.top_k(score, topk)
        valid = sel <= qpos[None, :, None]
        c_sel = jax.vmap(lambda cb, ib: cb[ib])(c, sel)
        logits = jnp.einsum('bqhc,bqkc->bqhk', qlb, c_sel).astype(jnp.float32)
        bias = bias_tab_f[rel_bucket(qpos[None, :, None] - sel)]
        logits = logits + jnp.moveaxis(bias, -1, 2)
        logits = jnp.where(valid[:, :, None, :], logits, NEG)
        p = jax.nn.softmax(logits, axis=-1).astype(c.dtype)
        o_lat = jnp.einsum('bqhk,bqkc->bqhc', p, c_sel)
        return jnp.einsum('bqhc,hcd->bqhd', o_lat, w_uv)

    out = lax.map(block_fn, (jnp.arange(nb), to_blocks(q_lat), to_blocks(q_idx), to_blocks(w_idx)))
    return jnp.moveaxis(out, 0, 1).reshape(B, L, DSA_HEADS * HEAD_DIM)


def dilated_group(q, k, v, window, dilation, bias_tab):
    B, L, H, dh = q.shape
    w = window // dilation
    ls = L // dilation
    nb = -(-ls // w)
    pad = nb * w - ls

    def to_sub(a):
        a = a.reshape(B, ls, dilation, H, dh).transpose(0, 2, 1, 3, 4)
        a = jnp.pad(a, ((0, 0), (0, 0), (0, pad), (0, 0), (0, 0)))
        return a.reshape(B, dilation, nb, w, H, dh)

    def with_prev(a):
        prev = jnp.pad(a, ((0, 0), (0, 0), (1, 0), (0, 0), (0, 0), (0, 0)))[:, :, :-1]
        return jnp.concatenate([prev, a], axis=3)

    qs = to_sub(q)
    kb = with_prev(to_sub(k))
    vb = with_prev(to_sub(v))
    qf = jnp.arange(w)[:, None] + w
    kf = jnp.arange(2 * w)[None, :]
    step = qf - kf
    key_sub = jnp.arange(nb)[:, None, None] * w + kf[None] - w
    mask = (step >= 0)[None] & (step <= w)[None] & (key_sub >= 0)
    bias = bias_tab.astype(jnp.float32)[rel_bucket(step * dilation)]
    logits = jnp.einsum('brnqhd,brnkhd->brnhqk', qs, kb).astype(jnp.float32) * dh ** -0.5
    logits = logits + jnp.moveaxis(bias, -1, 0)[None, None, None]
    logits = jnp.where(mask[None, None, :, None], logits, NEG)
    m = jnp.max(logits, axis=-1, keepdims=True)
    e = jnp.exp(logits - m)
    s = jnp.sum(e, axis=-1, keepdims=True)
    o = jnp.einsum('brnhqk,brnkhd->brnqhd', (e / s).astype(v.dtype), vb)
    lse = (m + jnp.log(s))[..., 0]
    o = o.reshape(B, dilation, nb * w, H, dh)[:, :, :ls].transpose(0, 2, 1, 3, 4).reshape(B, L, H, dh)
    lse = lse.transpose(0, 1, 2, 4, 3).reshape(B, dilation, nb * w, H)[:, :, :ls]
    lse = lse.transpose(0, 2, 1, 3).reshape(B, L, H)
    return o, lse


def dilated_branch(dil, bias_tab):
    B, L = dil.shape[:2]
    qkv = dil.reshape(B, L, 3, N_DIL_GROUPS, DIL_HEADS_PER_GROUP, HEAD_DIM)
    outs, lses = [], []
    for g, (window, dilation) in enumerate(DIL_GROUPS):
        tab = bias_tab[:, DSA_HEADS + g * DIL_HEADS_PER_GROUP: DSA_HEADS + (g + 1) * DIL_HEADS_PER_GROUP]
        o, lse = dilated_group(qkv[:, :, 0, g], qkv[:, :, 1, g], qkv[:, :, 2, g], window, dilation, tab)
        outs.append(o)
        lses.append(lse)
    alpha = jax.nn.softmax(jnp.stack(lses, axis=0), axis=0)
    o = jnp.sum(alpha[..., None].astype(dil.dtype) * jnp.stack(outs, axis=0), axis=0)
    return o.reshape(B, L, DIL_OUT)


def hier_moe(h, w_rg, b_rg, w_re, b_re, w_gate, w_up, w_down):
    B, L, D = h.shape
    t = h.reshape(-1, D)
    n_tok = t.shape[0]
    g_logits = (t @ w_rg + b_rg).astype(jnp.float32)
    g_prob = jax.nn.softmax(g_logits, axis=-1)
    g_sel = jnp.argmax(g_logits, axis=-1)
    p_g = jnp.take_along_axis(g_prob, g_sel[:, None], axis=1)[:, 0]
    e_logits = (jnp.einsum('nd,gde->nge', t, w_re) + b_re).astype(jnp.float32)
    e_logits = jnp.take_along_axis(e_logits, g_sel[:, None, None], axis=1)[:, 0]
    top_vals, top_idx = lax.top_k(e_logits, TOP_K_INNER)
    weights = p_g[:, None] * jax.nn.softmax(top_vals, axis=-1)
    gid = g_sel[:, None] * EXPERTS_PER_GROUP + top_idx
    combine = jnp.zeros((n_tok, N_EXPERTS), jnp.float32).at[jnp.arange(n_tok)[:, None], gid].add(weights)
    combine = combine.astype(t.dtype)
    out = jnp.zeros_like(t)
    for e in range(N_EXPERTS):
        hid = jax.nn.silu(t @ w_gate[e]) * (t @ w_up[e])
        out = out + combine[:, e:e + 1] * (hid @ w_down[e])
    return out.reshape(B, L, D)


def setup_inputs(seed: int = 0) -> dict:
    key = jax.random.key(seed)
    ks = jax.random.split(key, 20)
    f32 = jnp.float32

    def nrm(k, shape, scale):
        return jax.random.normal(k, shape, f32) * scale

    def gain(k, shape):
        return jnp.ones(shape, f32) + 0.01 * jax.random.normal(k, shape, f32)

    return {
        "x": jax.random.normal(ks[0], (BATCH, SEQ, D_MODEL), f32),
        "attn_norm": gain(ks[1], (DEPTH, D_MODEL)),
        "w_in": nrm(ks[2], (DEPTH, D_MODEL, IN_COLS), D_MODEL ** -0.5),
        "kv_norm": gain(ks[3], (DEPTH, KV_LATENT)),
        "w_uk": nrm(ks[4], (DEPTH, DSA_HEADS, KV_LATENT, HEAD_DIM), KV_LATENT ** -0.5),
        "w_uv": nrm(ks[5], (DEPTH, DSA_HEADS, KV_LATENT, HEAD_DIM), KV_LATENT ** -0.5),
        "rel_bias": nrm(ks[6], (NUM_BUCKETS, N_REL_HEADS), 0.1),
        "w_branch_a": nrm(ks[7], (DEPTH, DSA_HEADS * HEAD_DIM, D_MODEL), (DSA_HEADS * HEAD_DIM) ** -0.5),
        "w_branch_b": nrm(ks[8], (DEPTH, DIL_OUT, D_MODEL), DIL_OUT ** -0.5),
        "w_out": nrm(ks[9], (DEPTH, D_MODEL, D_MODEL), D_MODEL ** -0.5),
        "ffn_norm": gain(ks[10], (DEPTH, D_MODEL)),
        "w_router_group": nrm(ks[11], (DEPTH, D_MODEL, N_GROUPS), D_MODEL ** -0.5),
        "b_router_group": nrm(ks[12], (DEPTH, N_GROUPS), 0.01),
        "w_router_expert": nrm(ks[13], (DEPTH, N_GROUPS, D_MODEL, EXPERTS_PER_GROUP), D_MODEL ** -0.5),
        "b_router_expert": nrm(ks[14], (DEPTH, N_GROUPS, EXPERTS_PER_GROUP), 0.01),
        "w_gate": nrm(ks[15], (DEPTH, N_EXPERTS, D_MODEL, EXPERT_FF), D_MODEL ** -0.5),
        "w_up": nrm(ks[16], (DEPTH, N_EXPERTS, D_MODEL, EXPERT_FF), D_MODEL ** -0.5),
        "w_down": nrm(ks[17], (DEPTH, N_EXPERTS, EXPERT_FF, D_MODEL), EXPERT_FF ** -0.5),
        "final_norm": gain(ks[18], (D_MODEL,)),
    }


def reference(x, attn_norm, w_in, kv_norm, w_uk, w_uv, rel_bias, w_branch_a, w_branch_b, w_out,
              ffn_norm, w_router_group, b_router_group, w_router_expert, b_router_expert,
              w_gate, w_up, w_down, final_norm):
    B, L, D = x.shape
    h = x
    for layer in range(DEPTH):
        u = rms_norm(h, attn_norm[layer])
        proj = u @ w_in[layer]
        q_a, c_kv, q_idx, k_idx, w_idx, dil, gates = jnp.split(proj, SPLIT_POINTS, axis=-1)
        c_kv = rms_norm(c_kv, kv_norm[layer])
        y_a = dsa_branch(q_a.reshape(B, L, DSA_HEADS, HEAD_DIM), c_kv,
                         q_idx.reshape(B, L, IDX_HEADS, IDX_DIM), k_idx, w_idx,
                         w_uk[layer], w_uv[layer], rel_bias[:, :DSA_HEADS])
        y_b = dilated_branch(dil, rel_bias)
        g = jax.nn.sigmoid(gates.reshape(B, L, N_BRANCHES, D).astype(jnp.float32)).astype(x.dtype)
        mixed = g[:, :, 0] * (y_a @ w_branch_a[layer]) + g[:, :, 1] * (y_b @ w_branch_b[layer])
        h = h + mixed @ w_out[layer]
        h = h + hier_moe(rms_norm(h, ffn_norm[layer]), w_router_group[layer], b_router_group[layer],
                         w_router_expert[layer], b_router_expert[layer],
                         w_gate[layer], w_up[layer], w_down[layer])
    return rms_norm(h, final_norm)
```

```python
import math
import numpy as np
import concourse.bass as bass
import concourse.mybir as mybir
from concourse.ap import AP
from concourse.bass_utils import run_bass_kernel_spmd

F32 = mybir.dt.float32
BF16 = mybir.dt.bfloat16
AF = mybir.ActivationFunctionType
ALU = mybir.AluOpType
AX = mybir.AxisListType

ENGS = ("pe", "act", "dve", "pool", "sp")

L = 2048
D = 1024
NT = 16
INC = 5444
C_QA, C_KV, C_QI, C_KI, C_WI, C_DIL, C_G = 0, 512, 768, 1024, 1088, 1092, 3396
NEGM = -30000.0
TOPK = 256
NBIS = 14
DIL = ((128, 1), (512, 4), (2048, 16))
STREAMS = (("dsa", 0, 8, 2175, 2048), ("g0", 8, 4, 383, 256), ("g1", 12, 4, 767, 640), ("g2", 16, 4, 2175, 2048))
FROW = 2176


class Res:
    __slots__ = ("w", "r")

    def __init__(self):
        self.w = None
        self.r = []


def RL(n):
    return [Res() for _ in range(n)]


class Op:
    __slots__ = ("eng", "fn", "deps", "is_dma", "signal", "sigval", "dsem", "dval", "prev_dma")

    def __init__(self, eng, fn, is_dma):
        self.eng = eng
        self.fn = fn
        self.deps = ()
        self.is_dma = is_dma
        self.signal = False
        self.sigval = 0
        self.dsem = None
        self.dval = 0
        self.prev_dma = None


class Ker:
    def __init__(self):
        self.ops = []

    def _rec(self, eng, fn, reads, writes, is_dma=False):
        op = Op(eng, fn, is_dma)
        i = len(self.ops)
        deps = set()
        for r in reads:
            if r.w is not None:
                deps.add(r.w)
        for w in writes:
            if w.w is not None:
                deps.add(w.w)
            deps.update(w.r)
        op.deps = tuple(deps)
        for r in reads:
            r.r.append(i)
        for w in writes:
            w.w = i
            w.r = []
        self.ops.append(op)
        return i

    def pe(self, fn, reads=(), writes=()):
        return self._rec("pe", fn, reads, writes)

    def act(self, fn, reads=(), writes=()):
        return self._rec("act", fn, reads, writes)

    def dve(self, fn, reads=(), writes=()):
        return self._rec("dve", fn, reads, writes)

    def pool(self, fn, reads=(), writes=()):
        return self._rec("pool", fn, reads, writes)

    def dma(self, q, fn, reads=(), writes=()):
        return self._rec(q, fn, reads, writes, is_dma=True)

    def emit(self, block, sems, dma_sems):
        ops = self.ops
        for op in ops:
            for d in op.deps:
                dop = ops[d]
                if dop.is_dma:
                    continue
                if dop.eng == "pe" and op.eng == "pe" and not op.is_dma:
                    continue
                dop.signal = True
        cnt = {e: 0 for e in ENGS}
        dcount = {}
        dlast = {}
        dn = {e: 0 for e in ENGS}
        for op in ops:
            if op.is_dma:
                pool = dma_sems[op.eng]
                k = dn[op.eng] % len(pool)
                dn[op.eng] += 1
                key = (op.eng, k)
                dcount[key] = dcount.get(key, 0) + 1
                op.dsem = key
                op.dval = 16 * dcount[key]
                op.prev_dma = dlast.get(key)
                dlast[key] = op
            elif op.signal:
                cnt[op.eng] += 1
                op.sigval = cnt[op.eng]
        per_eng = {e: [] for e in ENGS}
        for op in ops:
            per_eng[op.eng].append(op)

        def semof(key):
            if isinstance(key, tuple):
                return dma_sems[key[0]][key[1]]
            return sems[key]

        def run_engine(ename, engine):
            known = {}
            for op in per_eng[ename]:
                need = {}
                for d in op.deps:
                    dop = ops[d]
                    if dop.is_dma:
                        key, val = dop.dsem, dop.dval
                    else:
                        if dop.eng == "pe" and ename == "pe" and not op.is_dma:
                            continue
                        key, val = dop.eng, dop.sigval
                    if need.get(key, 0) < val:
                        need[key] = val
                if op.is_dma and op.prev_dma is not None:
                    key, val = op.prev_dma.dsem, op.prev_dma.dval
                    if need.get(key, 0) < val:
                        need[key] = val
                for key, val in need.items():
                    if known.get(key, 0) >= val:
                        continue
                    engine.wait_ge(semof(key), val)
                    known[key] = val
                inst = op.fn(engine)
                if op.is_dma:
                    inst.then_inc(semof(op.dsem), 16)
                elif op.signal:
                    inst.then_inc(sems[ename], 1)
            for key, dop in dlast.items():
                if key[0] == ename and known.get(key, 0) < dop.dval:
                    engine.wait_ge(semof(key), dop.dval)

        @block.tensor
        def _(e):
            run_engine("pe", e)

        @block.scalar
        def _(e):
            run_engine("act", e)

        @block.vector
        def _(e):
            run_engine("dve", e)

        @block.gpsimd
        def _(e):
            run_engine("pool", e)

        @block.sync
        def _(e):
            run_engine("sp", e)


def _bucket(d):
    n = np.maximum(d, 0)
    nf = np.maximum(n, 1).astype(np.float32)
    large = 16 + (np.log(nf / np.float32(16)) / np.float32(math.log(2048 / 16)) * np.float32(16)).astype(np.int32)
    large = np.minimum(large, 31)
    return np.where(n < 16, n, large)


def _onehot_tables():
    cols = []
    for name, h0, nh, flen, ncols in STREAMS:
        d = np.arange(flen) - 127
        if name == "dsa":
            valid = d >= 0
        else:
            W, r = DIL[int(name[1])]
            valid = (d >= 0) & (d <= W) & (d % r == 0)
        oh = np.zeros((33, flen), np.float32)
        b = _bucket(d)
        for i in range(flen):
            if valid[i]:
                oh[b[i], i] = 1.0
            else:
                oh[32, i] = 1.0
        cols.append(oh)
    return np.concatenate(cols, axis=1)


def _bl(a, n):
    return AP(a.tensor, a.offset, [list(x) for x in a.ap] + [[0, n]])


def _bmid(a, n):
    ap = [list(x) for x in a.ap]
    return AP(a.tensor, a.offset, [ap[0], [0, n]] + ap[1:])


def build_nc(NB, stop_after=None, NEXP=32, NQT=16, NOATT=False, debug=False):
    nc = bass.Bass("TRN2", target_bir_lowering=False)

    def din(name, shape, dt=F32):
        return nc.dram_tensor(name, list(shape), dt, kind="ExternalInput").ap()

    x = din("x", [NB, L, D])
    attn_norm = din("attn_norm", [1, D])
    w_in = din("w_in", [1, D, INC])
    kv_norm = din("kv_norm", [1, 256])
    w_uk = din("w_uk", [1, 8, 256, 64])
    w_uv = din("w_uv", [1, 8, 256, 64])
    rel_bias = din("rel_bias", [32, 20])
    w_branch_a = din("w_branch_a", [1, 512, D])
    w_branch_b = din("w_branch_b", [1, 256, D])
    w_out = din("w_out", [1, D, D])
    ffn_norm = din("ffn_norm", [1, D])
    w_rg = din("w_router_group", [1, D, 4])
    b_rg = din("b_router_group", [1, 4])
    w_re = din("w_router_expert", [1, 4, D, 8])
    b_re = din("b_router_expert", [1, 4, 8])
    w_gate = din("w_gate", [1, NEXP, D, 512])
    w_up = din("w_up", [1, NEXP, D, 512])
    w_down = din("w_down", [1, NEXP, 512, D])
    final_norm = din("final_norm", [D])
    ohtab = din("ohtab", [33, 5500])
    pow2 = din("pow2", [1, 32])
    y = nc.dram_tensor("y", [NB, L, D], F32, kind="ExternalOutput").ap()
    fd = nc.dram_tensor("fd_scratch", [20, FROW], BF16, kind="Internal").ap()
    if debug:
        dbg_h = nc.dram_tensor("dbg_h", [L, D], F32, kind="ExternalOutput").ap()
        dbg_comb = nc.dram_tensor("dbg_comb", [L, 32], F32, kind="ExternalOutput").ap()
        dbg_ya = nc.dram_tensor("dbg_ya", [512, L], BF16, kind="ExternalOutput").ap()
        dbg_yb = nc.dram_tensor("dbg_yb", [256, L], BF16, kind="ExternalOutput").ap()
        dbg_mix = nc.dram_tensor("dbg_mix", [1024, L], BF16, kind="ExternalOutput").ap()
        dbg_hn = nc.dram_tensor("dbg_hn", [1024, L], BF16, kind="ExternalOutput").ap()
        dbg_cT = nc.dram_tensor("dbg_cT", [128, 2 * L], BF16, kind="ExternalOutput").ap()
        dbg_vh = nc.dram_tensor("dbg_vh", [128, 16 * 8 * 65], BF16, kind="ExternalOutput").ap()
        dbg_kh = nc.dram_tensor("dbg_kh", [128, 4 * L], BF16, kind="ExternalOutput").ap()
        dbg_vd = nc.dram_tensor("dbg_vd", [128, 8 * 2048], BF16, kind="ExternalOutput").ap()
        dbg_qa = nc.dram_tensor("dbg_qa", [128, 4 * L], BF16, kind="ExternalOutput").ap()
        dbg_yaf = nc.dram_tensor("dbg_yaf", [128, 8 * 65], F32, kind="ExternalOutput").ap()
        dbg_p0 = nc.dram_tensor("dbg_p0", [128, 512], BF16, kind="ExternalOutput").ap()
        dbg_p1 = nc.dram_tensor("dbg_p1", [128, 512], BF16, kind="ExternalOutput").ap()
        dbg_qp = nc.dram_tensor("dbg_qp", [128, 1024], BF16, kind="ExternalOutput").ap()

    K = Ker()
    from contextlib import ExitStack
    with ExitStack() as es:
        ARENA_W = 52400
        arena = es.enter_context(nc.sbuf_tensor("arena", [128, ARENA_W], F32))
        psb = [es.enter_context(nc.psum_tensor(f"psb{i}", [128, 512], F32)) for i in range(8)]
        RP = RL(8)
        sems = {e: es.enter_context(nc.semaphore("s_" + e)) for e in ENGS}
        dsems = {e: [es.enter_context(nc.semaphore(f"d_{e}{i}")) for i in range(8)] for e in ("sp", "pool", "act")}
        dsems["pe"] = []
        dsems["dve"] = []
        block = es.enter_context(nc.Block())

        class Arena:
            def __init__(self, lo, hi):
                self.lo, self.hi, self.p = lo, hi, lo

            def f32(self, *shape):
                n = int(np.prod(shape))
                off = self.p
                self.p += n + (n & 1)
                assert self.p <= self.hi, ("arena overflow", self.p, self.hi)
                v = arena[:, off:off + n]
                return self._shape(v, shape)

            def bf16(self, *shape):
                n = int(np.prod(shape))
                w = (n + 1) // 2
                w += (w & 1)
                off = self.p
                self.p += w
                assert self.p <= self.hi, ("arena overflow", self.p, self.hi)
                v = arena[:, off:off + w].bitcast(BF16)[:, 0:n]
                return self._shape(v, shape)

            @staticmethod
            def _shape(v, shape):
                if len(shape) == 1:
                    return v
                if len(shape) == 2:
                    return v.rearrange("p (a b) -> p a b", a=shape[0])
                if len(shape) == 3:
                    return v.rearrange("p (a b c) -> p a b c", a=shape[0], b=shape[1])
                raise ValueError

            def reset(self):
                self.p = self.lo

        G = Arena(0, 4200)
        ident_f = G.f32(128)
        ident_b = G.bf16(128)
        J_b = G.bf16(128)
        zero_b = G.bf16(512)
        tmp_f = G.f32(128)
        gcol_attn = G.f32(8)
        gcol_ffn = G.f32(8)
        kvn_b = G.f32(256)
        fin_b = G.f32(1024)
        wr = G.f32(8, 36)
        rb_b = G.f32(36)
        wuk = G.bf16(2, 512)
        wuv = G.bf16(2, 512)
        tabA = G.f32(20)
        pow2_b = G.f32(32)
        tri = G.f32(128)
        sgb = G.f32(32)
        R1 = Arena(G.hi, G.hi + 16384)
        R3 = Arena(R1.hi, ARENA_W)
        RG = {n: Res() for n in ("ident_f", "ident_b", "J_b", "zero_b", "tmp_f", "gcol_attn", "gcol_ffn", "kvn_b",
                                 "fin_b", "wr", "rb_b", "wuk", "wuv", "tabA", "pow2_b", "fd", "tri", "sgb")}

        def barrier():
            last = {}
            dmas = {}
            for i, op in enumerate(K.ops):
                if op.is_dma:
                    dmas.setdefault(op.eng, []).append(i)
                else:
                    last[op.eng] = i
            deps = tuple(last.values())
            for q, lst in dmas.items():
                deps = deps + tuple(lst[-8:])
            for eng in ("pe", "act", "dve", "pool", "sp"):
                if eng == "pe":
                    continue
                op = Op(eng, (lambda e: e.nop()), False)
                op.deps = deps
                K.ops.append(op)
            op = Op("pe", (lambda e: e.nop()), False)
            op.deps = deps
            K.ops.append(op)

        K.pool(lambda e: e.memset(ident_f, 1.0), writes=[RG["ident_f"]])
        K.pool(lambda e: e.affine_select(out=ident_f, in_=ident_f, pattern=[[1, 128]], compare_op=ALU.is_equal,
                                         fill=0.0, base=0, channel_multiplier=-1),
               reads=[RG["ident_f"]], writes=[RG["ident_f"]])
        K.dve(lambda e: e.tensor_copy(out=ident_b, in_=ident_f), reads=[RG["ident_f"]], writes=[RG["ident_b"]])
        K.pool(lambda e: e.memset(tmp_f, 1.0), writes=[RG["tmp_f"]])
        K.pool(lambda e: e.affine_select(out=tmp_f, in_=tmp_f, pattern=[[1, 128]], compare_op=ALU.is_equal,
                                         fill=0.0, base=-127, channel_multiplier=1),
               reads=[RG["tmp_f"]], writes=[RG["tmp_f"]])
        K.dve(lambda e: e.tensor_copy(out=J_b, in_=tmp_f), reads=[RG["tmp_f"]], writes=[RG["J_b"]])
        K.pool(lambda e: e.memset(zero_b, 0.0), writes=[RG["zero_b"]])
        K.pool(lambda e: e.memset(tri, 0.0), writes=[RG["tri"]])
        for i_ in range(1, 17):
            K.pool(lambda e, i_=i_: e.memset(sgb[:, i_:i_ + 1], float(i_ * 128) - 511.5), writes=[RG["sgb"]])
        K.pool(lambda e: e.affine_select(out=tri, in_=tri, pattern=[[-1, 128]], compare_op=ALU.is_ge,
                                         fill=-1e30, base=0, channel_multiplier=1),
               reads=[RG["tri"]], writes=[RG["tri"]])
        K.dma("sp", lambda e: e.dma_start(out=gcol_attn, in_=attn_norm[0].rearrange("(c p) -> p c", p=128),
                                          allow_slow_non_contiguous=True), writes=[RG["gcol_attn"]])
        K.dma("sp", lambda e: e.dma_start(out=gcol_ffn, in_=ffn_norm[0].rearrange("(c p) -> p c", p=128),
                                          allow_slow_non_contiguous=True), writes=[RG["gcol_ffn"]])
        K.dma("sp", lambda e: e.dma_start(out=kvn_b, in_=AP(kv_norm.tensor, 0, [[0, 128], [1, 256]])),
              writes=[RG["kvn_b"]])
        K.dma("sp", lambda e: e.dma_start(out=fin_b, in_=AP(final_norm.tensor, 0, [[0, 128], [1, 1024]])),
              writes=[RG["fin_b"]])
        K.dma("sp", lambda e: e.dma_start(out=pow2_b, in_=AP(pow2.tensor, 0, [[0, 128], [1, 32]])),
              writes=[RG["pow2_b"]])
        K.dma("sp", lambda e: e.dma_start(out=wr[:, :, 0:4], in_=w_rg[0].rearrange("(c p) n -> p c n", p=128)),
              writes=[RG["wr"]])
        for g in range(4):
            K.dma("sp", lambda e, g=g: e.dma_start(out=wr[:, :, 4 + 8 * g:12 + 8 * g],
                                                   in_=w_re[0, g].rearrange("(c p) n -> p c n", p=128)),
                  writes=[RG["wr"]])
        K.dma("sp", lambda e: e.dma_start(out=rb_b[:, 0:4], in_=AP(b_rg.tensor, 0, [[0, 128], [1, 4]])),
              writes=[RG["rb_b"]])
        K.dma("sp", lambda e: e.dma_start(out=rb_b[:, 4:36], in_=AP(b_re.tensor, 0, [[0, 128], [1, 32]])),
              writes=[RG["rb_b"]])
        for cc in range(2):
            K.dma("pool", lambda e, cc=cc: e.dma_start(out=wuk[:, cc, :].rearrange("p (h d) -> p h d", h=8),
                                                       in_=w_uk[0, :, cc * 128:(cc + 1) * 128, :].rearrange("h p d -> p h d")),
                  writes=[RG["wuk"]])
            K.dma("pool", lambda e, cc=cc: e.dma_start(out=wuv[:, cc, :].rearrange("p (h d) -> p h d", h=8),
                                                       in_=w_uv[0, :, cc * 128:(cc + 1) * 128, :].rearrange("h p d -> p h d")),
                  writes=[RG["wuv"]])
        K.dma("sp", lambda e: e.dma_start(out=tabA[0:32, :], in_=rel_bias[:, :]), writes=[RG["tabA"]])
        K.pool(lambda e: e.memset(tabA[32:33, :], NEGM), writes=[RG["tabA"]])

        R3.reset()
        ohs = R3.f32(5500)
        fsb = R3.bf16(FROW)
        Rohs, Rfsb = Res(), Res()
        K.dma("sp", lambda e: e.dma_start(out=ohs[0:33, :], in_=ohtab[:, :]), writes=[Rohs])
        col0 = 0
        pi = 0
        for name, h0, nh, flen, ncols in STREAMS:
            for c0 in range(0, flen, 512):
                n = min(512, flen - c0)
                bk = pi % 2
                pi += 1
                K.pe(lambda e, bk=bk, h0=h0, nh=nh, a=col0 + c0, n=n: e.matmul(
                    psb[bk][0:nh, 0:n], lhsT=tabA[0:33, h0:h0 + nh], rhs=ohs[0:33, a:a + n], start=True, stop=True),
                    reads=[RG["tabA"], Rohs], writes=[RP[bk]])
                K.dve(lambda e, bk=bk, nh=nh, c0=c0, n=n: e.tensor_copy(out=fsb[0:nh, c0:c0 + n], in_=psb[bk][0:nh, 0:n]),
                      reads=[RP[bk]], writes=[Rfsb])
            K.dma("sp", lambda e, h0=h0, nh=nh, flen=flen: e.dma_start(out=fd[h0:h0 + nh, 0:flen], in_=fsb[0:nh, 0:flen]),
                  reads=[Rfsb], writes=[RG["fd"]])
            col0 += flen
        barrier()

        rot = {"ps": 0}

        def build_V(V, RV, si, stage, Rstage, banks):
            name, h0, nh, flen, ncols = STREAMS[si]
            for hl in range(nh):
                sb_i = hl % 2
                K.dma("sp", lambda e, sb_i=sb_i, hl=hl: e.dma_start(
                    out=stage[sb_i][:, 0:ncols], in_=AP(fd.tensor, (h0 + hl) * FROW, [[1, 128], [1, ncols]])),
                    reads=[RG["fd"]], writes=[Rstage[sb_i]])
                for c0 in range(0, ncols, 512):
                    n = min(512, ncols - c0)
                    bk = banks[rot["ps"] % len(banks)]
                    rot["ps"] += 1
                    K.pe(lambda e, bk=bk, sb_i=sb_i, c0=c0, n=n: e.matmul(
                        psb[bk][:, 0:n], lhsT=J_b, rhs=stage[sb_i][:, c0:c0 + n], start=True, stop=True),
                        reads=[RG["J_b"], Rstage[sb_i]], writes=[RP[bk]])
                    K.act(lambda e, bk=bk, hl=hl, c0=c0, n=n: e.copy(out=V[:, hl, c0:c0 + n], in_=psb[bk][:, 0:n]),
                          reads=[RP[bk]], writes=[RV])

        evt = {"i": 0}

        def evac_copy(out, in_, reads, writes, scale=None, force=None):
            evt["i"] += 1
            if (evt["i"] % 2 == 0 and force is None) or force == "act":
                if scale is None:
                    K.act(lambda e: e.copy(out=out, in_=in_), reads=reads, writes=writes)
                else:
                    K.act(lambda e: e.mul(out=out, in_=in_, mul=scale), reads=reads, writes=writes)
            else:
                if scale is None:
                    K.dve(lambda e: e.tensor_copy(out=out, in_=in_), reads=reads, writes=writes)
                else:
                    K.dve(lambda e: e.tensor_scalar(out=out, in0=in_, scalar1=scale, scalar2=None, op0=ALU.mult),
                          reads=reads, writes=writes)

        def load_w(Wt, RW, col0, ncols, dst0=0):
            K.dma("pool", lambda e: e.dma_start(out=Wt[:, :, dst0:dst0 + ncols],
                                                in_=w_in[0, :, col0:col0 + ncols].rearrange("(c p) n -> p c n", p=128)),
                  writes=[RW])

        def proj_fm(Wt, RW, wcol0, M, uT, RuT, dst_fn, Rdst, banks, scale=None):
            for tb in range(4):
                bk = banks[rot["ps"] % len(banks)]
                rot["ps"] += 1
                for c in range(8):
                    K.pe(lambda e, bk=bk, c=c, tb=tb: e.matmul(
                        psb[bk][0:M, :], lhsT=Wt[:, c, wcol0:wcol0 + M], rhs=uT[:, c, tb * 512:(tb + 1) * 512],
                        start=(c == 0), stop=(c == 7)),
                        reads=[RW] + RuT[tb * 4:(tb + 1) * 4], writes=[RP[bk]])
                evac_copy(dst_fn(tb), psb[bk][0:M, :], [RP[bk]], [Rdst[tb]], scale)

        def rms_rstd(ss, rstd, Rss, Rrstd, n):
            K.dve(lambda e: e.tensor_scalar(out=rstd, in0=ss, scalar1=1.0 / n, scalar2=1e-6, op0=ALU.mult, op1=ALU.add),
                  reads=[Rss], writes=[Rrstd])
            K.act(lambda e: e.activation(out=rstd, in_=rstd, func=AF.Sqrt), reads=[Rrstd], writes=[Rrstd])
            K.dve(lambda e: e.reciprocal(out=rstd, in_=rstd), reads=[Rrstd], writes=[Rrstd])

        def attention(nheads, qT, RqT, kT, RkT, vv, Rvv, V, RV, dmax, qt, maskT, RmaskT, lg_banks, pv_banks,
                      finalize, psb_sb, Rpsb_sb, qpads):
            nhalf = nheads // 4
            qp, Rqp = qpads[qt % 2]
            nj = nheads // 2
            qpv = qp.rearrange("p (j two) t -> p j two t", two=2)
            K.pool(lambda e: e.tensor_copy(out=qpv[0:64, :, 0, :], in_=qT[0:64, 0:nj, qt * 128:(qt + 1) * 128]),
                   reads=[RqT[qt // 4]], writes=[Rqp])
            K.pool(lambda e: e.tensor_copy(out=qpv[64:128, :, 1, :], in_=qT[64:128, 0:nj, qt * 128:(qt + 1) * 128]),
                   reads=[RqT[qt // 4]], writes=[Rqp])
            for hf in range(nhalf):
                K.pe(lambda e, hf=hf: e.matmul(psb[pv_banks[hf]][:, 0:260], lhsT=zero_b[:, 0:128], rhs=zero_b[:, 0:260],
                                               start=True, stop=False, skip_group_check=True),
                     reads=[RG["zero_b"]], writes=[RP[pv_banks[hf]]])
            kts = list(range(max(0, qt - dmax), qt + 1))
            pend = None

            def do_pv(item):
                kt, pbufs = item
                for hf in range(nhalf):
                    for hh in range(4):
                        h = hf * 4 + hh
                        K.pe(lambda e, hf=hf, hh=hh, h=h, kt=kt, pb=pbufs[hf]: e.matmul(
                            psb[pv_banks[hf]][:, hh * 65:hh * 65 + 65], lhsT=psb_sb[pb][:, hh * 128:(hh + 1) * 128],
                            rhs=vv[:, kt, h, :], start=False, stop=(kt == qt), skip_group_check=True),
                            reads=[Rpsb_sb[pbufs[hf]], Rvv[kt]], writes=[RP[pv_banks[hf]]])

            for kt in kts:
                dl = qt - kt
                pbufs = []
                for hf in range(nhalf):
                    bk = lg_banks[rot["lg"] % len(lg_banks)]
                    pb = rot["lg"] % len(psb_sb)
                    rot["lg"] += 1
                    pbufs.append(pb)
                    K.pe(lambda e, bk=bk, hf=hf, dl=dl: e.matmul(
                        psb[bk][:, :].rearrange("p (h t) -> p h t", h=4), lhsT=ident_b,
                        rhs=V[:, hf * 4:hf * 4 + 4, dl * 128:(dl + 1) * 128], start=True, stop=False,
                        skip_group_check=True),
                        reads=[RG["ident_b"], RV], writes=[RP[bk]])
                    if maskT is not None:
                        K.pe(lambda e, bk=bk, kt=kt: e.matmul(
                            psb[bk][:, :].rearrange("p (h t) -> p h t", h=4), lhsT=ident_b,
                            rhs=_bmid(maskT[:, kt, :], 4), start=False, stop=False, skip_group_check=True),
                            reads=[RG["ident_b"], RmaskT], writes=[RP[bk]])
                    for hh in range(4):
                        h = hf * 4 + hh
                        j = h // 2
                        K.pe(lambda e, bk=bk, hh=hh, j=j, h=h, kt=kt: e.matmul(
                            psb[bk][:, hh * 128:(hh + 1) * 128], lhsT=kT[:, j, kt * 128:(kt + 1) * 128],
                            rhs=qp[:, h, :], start=False, stop=(hh == 3), skip_group_check=True),
                            reads=[RkT[kt // 4], Rqp], writes=[RP[bk]])
                    K.act(lambda e, bk=bk, pb=pb: e.activation(out=psb_sb[pb], in_=psb[bk][:, :], func=AF.Exp),
                          reads=[RP[bk]], writes=[Rpsb_sb[pb]])
                if pend is not None:
                    do_pv(pend)
                pend = (kt, pbufs)
            do_pv(pend)
            finalize()

        rot["lg"] = 0

        for b in range(NB):
            if stop_after == 'S':
                break
            R1.reset()
            R3.reset()
            uT = R1.bf16(8, L)
            y_aT = R1.bf16(4, L)
            y_bT = R1.bf16(2, L)
            RuT = RL(16)
            Ry_aT = RL(4)
            Ry_bT = RL(4)
            xst = [R3.f32(1024) for _ in range(3)]
            xn = [R3.bf16(1024) for _ in range(2)]
            sqj = R3.bf16(1024)
            ssA = [R3.f32(1) for _ in range(3)]
            rsA = [R3.f32(1) for _ in range(3)]
            Rxst, Rxn, RssA, RrsA, Rsqj = RL(3), RL(2), RL(3), RL(3), Res()
            for tt in range(NT):
                i3, i2 = tt % 3, tt % 2
                K.dma("sp", lambda e, i3=i3, tt=tt, b=b: e.dma_start(out=xst[i3], in_=x[b, tt * 128:(tt + 1) * 128, :]),
                      writes=[Rxst[i3]])
                K.act(lambda e, i3=i3: e.activation(out=sqj, in_=xst[i3], func=AF.Square, accum_out=ssA[i3]),
                      reads=[Rxst[i3]], writes=[Rsqj, RssA[i3]])
                rms_rstd(ssA[i3], rsA[i3], RssA[i3], RrsA[i3], 1024)
                K.act(lambda e, i3=i3, i2=i2: e.activation(out=xn[i2], in_=xst[i3], func=AF.Copy, scale=rsA[i3][:, 0:1]),
                      reads=[Rxst[i3], RrsA[i3]], writes=[Rxn[i2]])
                bk = tt % 2
                pT = psb[bk][:, :].bitcast(BF16)[:, 0:1024].rearrange("p (c t) -> p c t", c=8)
                for c in range(8):
                    K.pe(lambda e, c=c, i2=i2, pT=pT: e.transpose(out=pT[:, c, :], in_=xn[i2][:, c * 128:(c + 1) * 128],
                                                                  identity=ident_b),
                         reads=[Rxn[i2], RG["ident_b"]], writes=[RP[bk]])
                K.dve(lambda e, pT=pT, tt=tt: e.tensor_tensor(out=uT[:, :, tt * 128:(tt + 1) * 128], in0=pT,
                                                              in1=_bl(gcol_attn, 128), op=ALU.mult),
                      reads=[RP[bk], RG["gcol_attn"]], writes=[RuT[tt]])
            barrier()

            if stop_after == 'A':
                break
            R3.reset()
            q_aT = R3.bf16(4, L)
            q_iT = R1.bf16(2, L)
            k_iT = y_bT
            k_hT = R3.bf16(4, L)
            v_h = R3.bf16(NT, 8, 65)
            Vd = R3.bf16(8, 2048)
            score = R3.f32(2048)
            junk = R3.bf16(2048)
            cT = score.bitcast(BF16).rearrange("p (c t) -> p c t", c=2)
            mask_b = R3.bf16(2048)
            maskT = [R3.bf16(NT, 128) for _ in range(2)]
            p_sb = [R3.bf16(512) for _ in range(4)]
            Wst = [m.rearrange("p a b -> p (a b)").rearrange("p (c n) -> p c n", c=8) for m in maskT]
            Vst = [junk, mask_b]
            c_tok = [R3.bf16(256) for _ in range(2)]
            rl = [R3.f32(512) for _ in range(2)]
            absw = R3.f32(NT, 4)
            sgn = R3.f32(NT, 4)
            ssB = [R3.f32(1) for _ in range(2)]
            rsB = [R3.f32(1) for _ in range(2)]
            sm = R3.f32(8)
            sB = R3.f32(1)
            junkB = junk
            RsB = Res()
            stepT = R3.f32(32)
            ya_f = R3.f32(8, 65)
            rec8 = R3.f32(8)
            ya_b = R3.bf16(512)
            qpads = [(R3.bf16(8, 128), Res()) for _ in range(2)]
            for qp_, Rqp_ in qpads:
                K.pool(lambda e, qp_=qp_: e.memset(qp_, 0.0), writes=[Rqp_])
            Rq_aT, Rq_iT, Rk_iT, Rk_hT = RL(4), RL(4), RL(4), RL(4)
            Rv_h = RL(NT)
            RVd, Rscore, Rjunk, Rmask_b = Res(), Res(), Res(), Res()
            RjunkB = Rjunk
            RcT = Rscore
            RmaskT, Rp_sb, Rc_tok, Rrl = RL(2), RL(4), RL(2), RL(2)
            RWst = RmaskT
            RVst = [Rjunk, Rmask_b]
            Rabsw, Rsgn, RssB, RrsB = Res(), Res(), RL(2), RL(2)
            Rsm, RstepT, Rya_f, Rrec8, Rya_b = Res(), Res(), Res(), Res(), Res()

            build_V(Vd, RVd, 0, Vst, RVst, [0, 1])
            if stop_after == 'B1':
                break
            K.pool(lambda e: e.memset(v_h[:, :, :, 64:65], 1.0), writes=Rv_h)
            for j in range(4):
                wi = j % 2
                load_w(Wst[wi], RWst[wi], C_QA + j * 128, 128)
                proj_fm(Wst[wi], RWst[wi], 0, 128, uT, RuT, lambda tb, j=j: q_aT[:, j, tb * 512:(tb + 1) * 512],
                        Rq_aT, [0, 1])
            for j in range(2):
                wi = j % 2
                load_w(Wst[wi], RWst[wi], C_QI + j * 128, 128)
                proj_fm(Wst[wi], RWst[wi], 0, 128, uT, RuT, lambda tb, j=j: q_iT[:, j, tb * 512:(tb + 1) * 512],
                        Rq_iT, [0, 1])
            for v in range(2):
                K.pool(lambda e, v=v: e.memset(Wst[v][:, :, 0:128], 0.0), writes=[RWst[v]])
                load_w(Wst[v], RWst[v], C_KI, 64, 64 * v)
                proj_fm(Wst[v], RWst[v], 0, 128, uT, RuT, lambda tb, v=v: k_iT[:, v, tb * 512:(tb + 1) * 512], Rk_iT, [0, 1])
            if stop_after == 'B2':
                break
            load_w(Wst[1], RWst[1], C_KV, 256, 0)
            Wx = R3.bf16(8, 4)
            RWx = Res()
            load_w(Wx, RWx, C_WI, 4, 0)
            for tt in range(NT):
                bk = tt % 2
                i2 = tt % 2
                for c in range(8):
                    K.pe(lambda e, bk=bk, c=c, tt=tt: e.matmul(psb[bk][:, 0:256], lhsT=uT[:, c, tt * 128:(tt + 1) * 128],
                                                               rhs=Wst[1][:, c, 0:256], start=(c == 0), stop=(c == 7)),
                         reads=[RuT[tt], RWst[1]], writes=[RP[bk]])
                for c in range(8):
                    K.pe(lambda e, bk=bk, c=c, tt=tt: e.matmul(psb[bk][:, 256:260], lhsT=uT[:, c, tt * 128:(tt + 1) * 128],
                                                               rhs=Wx[:, c, 0:4], start=(c == 0), stop=(c == 7),
                                                               skip_group_check=True),
                         reads=[RuT[tt], RWx], writes=[RP[bk]])
                K.act(lambda e, bk=bk, i2=i2: e.activation(out=junk[:, 0:256], in_=psb[bk][:, 0:256], func=AF.Square,
                                                           accum_out=ssB[i2]),
                      reads=[RP[bk]], writes=[Rjunk, RssB[i2]])
                rms_rstd(ssB[i2], rsB[i2], RssB[i2], RrsB[i2], 256)
                K.dve(lambda e, bk=bk, i2=i2: e.scalar_tensor_tensor(out=c_tok[i2], in0=psb[bk][:, 0:256],
                                                                     scalar=rsB[i2][:, 0:1], in1=kvn_b,
                                                                     op0=ALU.mult, op1=ALU.mult),
                      reads=[RP[bk], RrsB[i2], RG["kvn_b"]], writes=[Rc_tok[i2]])
                K.act(lambda e, bk=bk, tt=tt: e.activation(out=absw[:, tt, :], in_=psb[bk][:, 256:260], func=AF.Abs,
                                                           scale=0.0625),
                      reads=[RP[bk]], writes=[Rabsw])
                K.dve(lambda e, bk=bk, tt=tt: e.tensor_scalar(out=sgn[:, tt, :], in0=psb[bk][:, 256:260], scalar1=0.0,
                                                              scalar2=2.0, op0=ALU.is_gt, op1=ALU.mult),
                      reads=[RP[bk]], writes=[Rsgn])
                K.dve(lambda e, tt=tt: e.tensor_scalar(out=sgn[:, tt, :], in0=sgn[:, tt, :], scalar1=-1.0, scalar2=None,
                                                       op0=ALU.add),
                      reads=[Rsgn], writes=[Rsgn])
                bk2 = 2 + tt % 2
                pT2 = psb[bk2][:, :].bitcast(BF16)[:, 0:256].rearrange("p (c t) -> p c t", c=2)
                for cc in range(2):
                    K.pe(lambda e, cc=cc, i2=i2, pT2=pT2: e.transpose(out=pT2[:, cc, :],
                                                                      in_=c_tok[i2][:, cc * 128:(cc + 1) * 128],
                                                                      identity=ident_b),
                         reads=[Rc_tok[i2], RG["ident_b"]], writes=[RP[bk2]])
                evac_copy(cT[:, :, tt * 128:(tt + 1) * 128], pT2, [RP[bk2]], [RcT])
            if stop_after == 'B3':
                break
            for j in range(4):
                for kb in range(4):
                    bk = rot["ps"] % 2
                    rot["ps"] += 1
                    for cc in range(2):
                        K.pe(lambda e, bk=bk, cc=cc, j=j, kb=kb: e.matmul(
                            psb[bk][:, :], lhsT=wuk[:, cc, j * 128:(j + 1) * 128], rhs=cT[:, cc, kb * 512:(kb + 1) * 512],
                            start=(cc == 0), stop=(cc == 1)), reads=[RG["wuk"], RcT], writes=[RP[bk]])
                    evac_copy(k_hT[:, j, kb * 512:(kb + 1) * 512], psb[bk][:, :], [RP[bk]], [Rk_hT[kb]], 0.125)
            for tt in range(NT):
                bk = rot["ps"] % 2
                rot["ps"] += 1
                for cc in range(2):
                    K.pe(lambda e, bk=bk, cc=cc, tt=tt: e.matmul(
                        psb[bk][:, :], lhsT=cT[:, cc, tt * 128:(tt + 1) * 128], rhs=wuv[:, cc, :],
                        start=(cc == 0), stop=(cc == 1)), reads=[RG["wuv"], RcT], writes=[RP[bk]])
                evac_copy(v_h[:, tt, :, 0:64], psb[bk][:, :].rearrange("p (h d) -> p h d", h=8), [RP[bk]], [Rv_h[tt]])

            if debug and b == 0:
                K.dma("sp", lambda e: e.dma_start(out=dbg_cT, in_=cT.rearrange("p c t -> p (c t)")), reads=[RcT])
                K.dma("sp", lambda e: e.dma_start(out=dbg_vh, in_=v_h.rearrange("p a b c -> p (a b c)")), reads=Rv_h)
                K.dma("sp", lambda e: e.dma_start(out=dbg_kh, in_=k_hT.rearrange("p c t -> p (c t)")), reads=Rk_hT)
                K.dma("sp", lambda e: e.dma_start(out=dbg_vd, in_=Vd.rearrange("p c t -> p (c t)")), reads=[RVd])
                K.dma("sp", lambda e: e.dma_start(out=dbg_qa, in_=q_aT.rearrange("p c t -> p (c t)")), reads=Rq_aT)
            if stop_after == 'B4':
                break
            def stA1(qt):
                nk = (qt + 1) * 128
                if True:
                    for kb in range((nk + 511) // 512):
                        ncol = min(512, nk - kb * 512)
                        for h in range(4):
                            bk = rot["ps"] % 2
                            rot["ps"] += 1
                            j, pbase = h // 2, (h % 2) * 64
                            ri = rot["ps"] % 2
                            K.pe(lambda e, bk=bk, j=j, h=h, kb=kb, ncol=ncol, qt=qt: e.matmul(
                                psb[bk][:, 0:ncol], lhsT=q_iT[:, j, qt * 128:(qt + 1) * 128],
                                rhs=k_iT[:, h % 2, kb * 512:kb * 512 + ncol], start=True, stop=True),
                                reads=[Rq_iT[qt // 4], Rk_iT[kb]], writes=[RP[bk]])
                            K.act(lambda e, bk=bk, ri=ri, ncol=ncol, qt=qt, h=h: e.activation(
                                out=rl[ri][:, 0:ncol], in_=psb[bk][:, 0:ncol], func=AF.Relu, scale=absw[:, qt, h:h + 1]),
                                reads=[RP[bk], Rabsw], writes=[Rrl[ri]])
                            if h == 0:
                                K.dve(lambda e, ri=ri, kb=kb, ncol=ncol, qt=qt: e.tensor_scalar(
                                    out=score[:, kb * 512:kb * 512 + ncol], in0=rl[ri][:, 0:ncol],
                                    scalar1=sgn[:, qt, 0:1], scalar2=None, op0=ALU.mult),
                                    reads=[Rrl[ri], Rsgn], writes=[Rscore])
                            else:
                                K.dve(lambda e, ri=ri, kb=kb, ncol=ncol, qt=qt, h=h: e.scalar_tensor_tensor(
                                    out=score[:, kb * 512:kb * 512 + ncol], in0=rl[ri][:, 0:ncol],
                                    scalar=sgn[:, qt, h:h + 1], in1=score[:, kb * 512:kb * 512 + ncol],
                                    op0=ALU.mult, op1=ALU.add),
                                    reads=[Rrl[ri], Rsgn, Rscore], writes=[Rscore])
                    K.pool(lambda e, qt=qt: e.tensor_tensor(
                        out=score[:, qt * 128:(qt + 1) * 128], in0=score[:, qt * 128:(qt + 1) * 128], in1=tri, op=ALU.add),
                        reads=[Rscore, RG["tri"]], writes=[Rscore])
                    nc0 = qt * 128
                    K.dve(lambda e, nk=nk: e.tensor_reduce(out=sm[:, 0:1], in_=score[:, 0:nk], axis=AX.X, op=ALU.max),
                          reads=[Rscore], writes=[Rsm])
                    K.dve(lambda e: e.tensor_reduce(out=sm[:, 1:2], in_=score[:, 0:256], axis=AX.X, op=ALU.min),
                          reads=[Rscore], writes=[Rsm])
                    K.dve(lambda e: e.tensor_scalar(out=sm[:, 1:2], in0=sm[:, 1:2], scalar1=-1.0, scalar2=None, op0=ALU.add),
                          reads=[Rsm], writes=[Rsm])
                    K.dve(lambda e: e.tensor_tensor(out=sm[:, 2:3], in0=sm[:, 0:1], in1=sm[:, 1:2], op=ALU.subtract),
                          reads=[Rsm], writes=[Rsm])
                    K.dve(lambda e: e.tensor_scalar(out=stepT, in0=pow2_b, scalar1=sm[:, 2:3], scalar2=None, op0=ALU.mult),
                          reads=[Rsm, RG["pow2_b"]], writes=[RstepT])
                    K.dve(lambda e: e.tensor_tensor(out=sm[:, 5:6], in0=sm[:, 1:2], in1=stepT[:, 1:2], op=ALU.add),
                          reads=[Rsm, RstepT], writes=[Rsm])
                    for k in range(1, NBIS + 1):
                        K.dve(lambda e, nk=nk: e.tensor_scalar(out=junk[:, 0:nk], in0=score[:, 0:nk], scalar1=sm[:, 5:6],
                                                               scalar2=None, op0=ALU.is_gt, op1=ALU.add,
                                                               accum_out=sm[:, 3:4]),
                              reads=[Rscore, Rsm], writes=[Rjunk, Rsm])
                        K.dve(lambda e, k=k: e.tensor_scalar(out=sm[:, 4:5], in0=sm[:, 3:4], scalar1=float(TOPK),
                                                             scalar2=stepT[:, k:k + 1], op0=ALU.is_ge, op1=ALU.mult),
                              reads=[Rsm, RstepT], writes=[Rsm])
                        kk = k + 1 if k < NBIS else k
                        K.dve(lambda e, kk=kk: e.scalar_tensor_tensor(out=sm[:, 5:6], in0=sm[:, 4:5],
                                                                      scalar=stepT[:, kk:kk + 1], in1=sm[:, 5:6],
                                                                      op0=ALU.subtract, op1=ALU.add),
                              reads=[Rsm, RstepT], writes=[Rsm])
                    K.dve(lambda e, nk=nk: e.tensor_scalar(out=mask_b[:, 0:nk], in0=score[:, 0:nk], scalar1=sm[:, 5:6],
                                                           scalar2=NEGM, op0=ALU.is_le, op1=ALU.mult),
                          reads=[Rscore, Rsm], writes=[Rmask_b])
            def stA2(qt):
                mT = maskT[qt % 2]
                RmT = RmaskT[qt % 2]
                if True:
                    for k0 in range(0, qt + 1, 4):
                        nb_ = min(4, qt + 1 - k0)
                        bk = (rot["ps"] % 2)
                        rot["ps"] += 1
                        pT3 = psb[bk][:, :].bitcast(BF16)[:, 0:512].rearrange("p (c t) -> p c t", c=4)
                        for i in range(nb_):
                            K.pe(lambda e, pT3=pT3, i=i, k0=k0: e.transpose(
                                out=pT3[:, i, :], in_=mask_b[:, (k0 + i) * 128:(k0 + i + 1) * 128], identity=ident_b),
                                reads=[Rmask_b, RG["ident_b"]], writes=[RP[bk]])
                        evac_copy(mT[:, k0:k0 + nb_, :], pT3[:, 0:nb_, :], [RP[bk]], [RmT], force="dve")

            def stB(qt):
                mT = maskT[qt % 2]
                RmT = RmaskT[qt % 2]
                def fin_dsa(qt=qt):
                    for hf in range(2):
                        K.act(lambda e, hf=hf: e.copy(out=ya_f[:, hf * 4:hf * 4 + 4, :],
                                                      in_=psb[6 + hf][:, 0:260].rearrange("p (h d) -> p h d", h=4)),
                              reads=[RP[6 + hf]], writes=[Rya_f])
                    K.dve(lambda e: e.reciprocal(out=rec8, in_=ya_f[:, :, 64]), reads=[Rya_f], writes=[Rrec8])
                    K.dve(lambda e: e.tensor_tensor(out=ya_b.rearrange("p (h d) -> p h d", h=8), in0=ya_f[:, :, 0:64],
                                                    in1=_bl(rec8, 64), op=ALU.mult),
                          reads=[Rya_f, Rrec8], writes=[Rya_b])
                    bk = (rot["ps"] % 2)
                    rot["ps"] += 1
                    pT4 = psb[bk][:, :].bitcast(BF16)[:, 0:512].rearrange("p (c t) -> p c t", c=4)
                    for j in range(4):
                        K.pe(lambda e, pT4=pT4, j=j: e.transpose(out=pT4[:, j, :], in_=ya_b[:, j * 128:(j + 1) * 128],
                                                                 identity=ident_b),
                             reads=[Rya_b, RG["ident_b"]], writes=[RP[bk]])
                    evac_copy(y_aT[:, :, qt * 128:(qt + 1) * 128], pT4, [RP[bk]], [Ry_aT[qt // 4]], force="dve")

                if not NOATT:
                  attention(8, q_aT, Rq_aT, k_hT, Rk_hT, v_h, Rv_h, Vd, RVd, 15, qt,
                          mT if qt >= 2 else None, RmT, [2, 3, 4, 5], [6, 7], fin_dsa, p_sb, Rp_sb, qpads)
            for qt in range(NQT):
                if 2 <= qt + 1 < NQT:
                    stA1(qt + 1)
                stB(qt)
                if 2 <= qt + 1 < NQT:
                    stA2(qt + 1)
            barrier()

            if debug and b == 0:
                K.dma("sp", lambda e: e.dma_start(out=dbg_yaf, in_=ya_f.rearrange("p a b -> p (a b)")), reads=[Rya_f])
                K.dma("sp", lambda e: e.dma_start(out=dbg_p0, in_=p_sb[0]), reads=[Rp_sb[0]])
                K.dma("sp", lambda e: e.dma_start(out=dbg_p1, in_=p_sb[1]), reads=[Rp_sb[1]])
                K.dma("sp", lambda e: e.dma_start(out=dbg_qp, in_=qpads[0][0].rearrange("p a b -> p (a b)")), reads=[qpads[0][1]])
                K.dma("sp", lambda e: e.dma_start(out=dbg_ya.rearrange("(c p) t -> p c t", p=128), in_=y_aT), reads=Ry_aT)
            if stop_after == 'B':
                break
            R3.reset()
            acc = R3.f32(NT, 260)
            dq = R3.bf16(2, L)
            dk = R3.bf16(2, L)
            dv = R3.bf16(NT, 4, 65)
            Vg = R3.bf16(4, 2048)
            p_sbC = [R3.bf16(512) for _ in range(4)]
            WstC = [R3.bf16(8, 256) for _ in range(2)]
            VstC = [R3.bf16(2048) for _ in range(2)]
            yb_f = R3.f32(4, 65)
            rec4 = R3.f32(4)
            yb_b = R3.bf16(256)
            qpadsC = [(R3.bf16(4, 128), Res()) for _ in range(2)]
            for qp_, Rqp_ in qpadsC:
                K.pool(lambda e, qp_=qp_: e.memset(qp_, 0.0), writes=[Rqp_])
            Racc = RL(NT)
            Rdq, Rdk = RL(4), RL(4)
            Rdv = RL(NT)
            RVg = Res()
            Rp_sbC, RWstC, RVstC = RL(4), RL(2), RL(2)
            Ryb_f, Rrec4, Ryb_b = Res(), Res(), Res()
            for g in range(3):
                W_, r_ = DIL[g]
                dmax = W_ // 128
                build_V(Vg, RVg, 1 + g, VstC, RVstC, [0, 1])
                K.pool(lambda e: e.memset(dv[:, :, :, 64:65], 1.0), writes=Rdv)
                for j in range(2):
                    load_w(WstC[0], RWstC[0], C_DIL + g * 256 + j * 128, 128)
                    proj_fm(WstC[0], RWstC[0], 0, 128, uT, RuT, lambda tb, j=j: dq[:, j, tb * 512:(tb + 1) * 512], Rdq,
                            [0, 1])
                    load_w(WstC[1], RWstC[1], C_DIL + 768 + g * 256 + j * 128, 128)
                    proj_fm(WstC[1], RWstC[1], 0, 128, uT, RuT, lambda tb, j=j: dk[:, j, tb * 512:(tb + 1) * 512], Rdk,
                            [0, 1], 0.125)
                load_w(WstC[0], RWstC[0], C_DIL + 1536 + g * 256, 256)
                for tt in range(NT):
                    bk = rot["ps"] % 2
                    rot["ps"] += 1
                    for c in range(8):
                        K.pe(lambda e, bk=bk, c=c, tt=tt: e.matmul(psb[bk][:, 0:256], lhsT=uT[:, c, tt * 128:(tt + 1) * 128],
                                                                   rhs=WstC[0][:, c, 0:256], start=(c == 0), stop=(c == 7)),
                             reads=[RuT[tt], RWstC[0]], writes=[RP[bk]])
                    evac_copy(dv[:, tt, :, 0:64], psb[bk][:, 0:256].rearrange("p (h d) -> p h d", h=4), [RP[bk]],
                              [Rdv[tt]])
                for qt in range(NT):
                    def fin_dil(qt=qt, g=g):
                        if g == 0:
                            K.act(lambda e: e.copy(out=acc[:, qt, :], in_=psb[6][:, 0:260]), reads=[RP[6]],
                                  writes=[Racc[qt]])
                        elif g == 1:
                            K.dve(lambda e: e.tensor_tensor(out=acc[:, qt, :], in0=psb[6][:, 0:260], in1=acc[:, qt, :],
                                                            op=ALU.add), reads=[RP[6], Racc[qt]], writes=[Racc[qt]])
                        else:
                            K.dve(lambda e: e.tensor_tensor(out=yb_f.rearrange("p h d -> p (h d)"), in0=psb[6][:, 0:260],
                                                            in1=acc[:, qt, :], op=ALU.add),
                                  reads=[RP[6], Racc[qt]], writes=[Ryb_f])
                            K.dve(lambda e: e.reciprocal(out=rec4, in_=yb_f[:, :, 64]), reads=[Ryb_f], writes=[Rrec4])
                            K.dve(lambda e: e.tensor_tensor(out=yb_b.rearrange("p (h d) -> p h d", h=4),
                                                            in0=yb_f[:, :, 0:64], in1=_bl(rec4, 64), op=ALU.mult),
                                  reads=[Ryb_f, Rrec4], writes=[Ryb_b])
                            bk = (rot["ps"] % 2)
                            rot["ps"] += 1
                            pT5 = psb[bk][:, :].bitcast(BF16)[:, 0:256].rearrange("p (c t) -> p c t", c=2)
                            for j in range(2):
                                K.pe(lambda e, pT5=pT5, j=j: e.transpose(out=pT5[:, j, :],
                                                                         in_=yb_b[:, j * 128:(j + 1) * 128],
                                                                         identity=ident_b),
                                     reads=[Ryb_b, RG["ident_b"]], writes=[RP[bk]])
                            evac_copy(y_bT[:, :, qt * 128:(qt + 1) * 128], pT5, [RP[bk]], [Ry_bT[qt // 4]])

                    attention(4, dq, Rdq, dk, Rdk, dv, Rdv, Vg, RVg, dmax, qt, None, None, [2, 3, 4, 5], [6], fin_dil,
                              p_sbC, Rp_sbC, qpadsC)
            barrier()

            if debug and b == 0:
                K.dma("sp", lambda e: e.dma_start(out=dbg_ya.rearrange("(c p) t -> p c t", p=128), in_=y_aT), reads=Ry_aT)
                K.dma("sp", lambda e: e.dma_start(out=dbg_yb.rearrange("(c p) t -> p c t", p=128), in_=y_bT), reads=Ry_bT)
            if stop_after == 'C':
                break
            R3.reset()
            mixT = R3.bf16(8, L)
            markD = R3.p
            Wg_ = [R3.bf16(8, 256) for _ in range(2)]
            Wa = R3.bf16(4, 1024)
            Wb = R3.bf16(2, 1024)
            sig = [R3.f32(512) for _ in range(4)]
            mm_ = [R3.f32(512) for _ in range(4)]
            RmixT = RL(16)
            RWg_, RWa, RWb, Rsig, Rmm_ = RL(2), Res(), Res(), RL(4), RL(4)
            K.dma("pool", lambda e: e.dma_start(out=Wa, in_=w_branch_a[0].rearrange("(c p) n -> p c n", p=128)),
                  writes=[RWa])
            K.dma("pool", lambda e: e.dma_start(out=Wb, in_=w_branch_b[0].rearrange("(c p) n -> p c n", p=128)),
                  writes=[RWb])
            it = 0
            for oc in range(8):
                wi = oc % 2
                load_w(Wg_[wi], RWg_[wi], C_G + oc * 128, 128, 0)
                load_w(Wg_[wi], RWg_[wi], C_G + 1024 + oc * 128, 128, 128)
                for tb in range(4):
                    s0, s1 = (it * 2) % 4, (it * 2 + 1) % 4
                    it += 1
                    for br in range(2):
                        bk = br
                        for c in range(8):
                            K.pe(lambda e, bk=bk, c=c, br=br, wi=wi, tb=tb: e.matmul(
                                psb[bk][:, :], lhsT=Wg_[wi][:, c, br * 128:(br + 1) * 128],
                                rhs=uT[:, c, tb * 512:(tb + 1) * 512], start=(c == 0), stop=(c == 7)),
                                reads=[RWg_[wi]] + RuT[tb * 4:(tb + 1) * 4], writes=[RP[bk]])
                        si = s0 if br == 0 else s1
                        K.act(lambda e, bk=bk, si=si: e.activation(out=sig[si], in_=psb[bk][:, :], func=AF.Sigmoid),
                              reads=[RP[bk]], writes=[Rsig[si]])
                    for f in range(4):
                        K.pe(lambda e, f=f, oc=oc, tb=tb: e.matmul(psb[2][:, :], lhsT=Wa[:, f, oc * 128:(oc + 1) * 128],
                                                                   rhs=y_aT[:, f, tb * 512:(tb + 1) * 512],
                                                                   start=(f == 0), stop=(f == 3)),
                             reads=[RWa, Ry_aT[tb]], writes=[RP[2]])
                    for f in range(2):
                        K.pe(lambda e, f=f, oc=oc, tb=tb: e.matmul(psb[3][:, :], lhsT=Wb[:, f, oc * 128:(oc + 1) * 128],
                                                                   rhs=y_bT[:, f, tb * 512:(tb + 1) * 512],
                                                                   start=(f == 0), stop=(f == 1)),
                             reads=[RWb, Ry_bT[tb]], writes=[RP[3]])
                    K.dve(lambda e, s0=s0: e.tensor_tensor(out=mm_[s0], in0=psb[2][:, :], in1=sig[s0], op=ALU.mult),
                          reads=[RP[2], Rsig[s0]], writes=[Rmm_[s0]])
                    K.dve(lambda e, s1=s1: e.tensor_tensor(out=mm_[s1], in0=psb[3][:, :], in1=sig[s1], op=ALU.mult),
                          reads=[RP[3], Rsig[s1]], writes=[Rmm_[s1]])
                    K.pool(lambda e, s0=s0, s1=s1, oc=oc, tb=tb: e.tensor_tensor(
                        out=mixT[:, oc, tb * 512:(tb + 1) * 512], in0=mm_[s0], in1=mm_[s1], op=ALU.add),
                        reads=[Rmm_[s0], Rmm_[s1]], writes=RmixT[tb * 4:(tb + 1) * 4])
            barrier()

            if debug and b == 0:
                K.dma("sp", lambda e: e.dma_start(out=dbg_mix.rearrange("(c p) t -> p c t", p=128), in_=mixT), reads=RmixT)
            if stop_after == 'D':
                break
            R1.reset()
            hh = R1.f32(NT, 1024)
            Rh = RL(NT)
            R3.p = markD
            hnT = R3.bf16(8, L)
            RhnT = RL(4)
            comb = R3.f32(NT, 32)
            Rcomb = RL(NT)
            markE = R3.p
            Wo = R3.bf16(8, 1024)
            RWo = Res()
            K.dma("pool", lambda e: e.dma_start(out=Wo, in_=w_out[0].rearrange("(c p) n -> p c n", p=128)), writes=[RWo])
            hn_f = [R3.f32(1024) for _ in range(2)]
            hnT_f = [R3.f32(8, 128) for _ in range(2)]
            ssE = [R3.f32(1) for _ in range(2)]
            rsE = [R3.f32(1) for _ in range(2)]
            lgA = R3.f32(NT, 36)
            elA = R3.f32(NT, 32)
            oh1 = R3.f32(NT, 32)
            oh2 = R3.f32(NT, 32)
            ex4 = R3.f32(NT, 4)
            pen = R3.f32(NT, 4)
            gm, pg, m1, m2, dd, w1p, w2p = [R3.f32(NT) for _ in range(7)]
            Rhn_f, RhnT_f, RssE, RrsE = RL(2), RL(2), RL(2), RL(2)
            RlgA, RelA, Roh1, Roh2, Rex4, Rpen, Rrt2 = Res(), Res(), Res(), Res(), Res(), Res(), Res()
            for tt in range(NT):
                i2 = tt % 2
                K.dma("sp", lambda e, tt=tt, b=b: e.dma_start(out=hh[:, tt, :], in_=x[b, tt * 128:(tt + 1) * 128, :]),
                      writes=[Rh[tt]])
                for nh in range(2):
                    bk = nh
                    for c in range(8):
                        K.pe(lambda e, bk=bk, c=c, tt=tt, nh=nh: e.matmul(
                            psb[bk][:, :], lhsT=mixT[:, c, tt * 128:(tt + 1) * 128], rhs=Wo[:, c, nh * 512:(nh + 1) * 512],
                            start=(c == 0), stop=(c == 7)), reads=[RmixT[tt], RWo], writes=[RP[bk]])
                    K.dve(lambda e, bk=bk, tt=tt, nh=nh: e.tensor_tensor(
                        out=hh[:, tt, nh * 512:(nh + 1) * 512], in0=psb[bk][:, :], in1=hh[:, tt, nh * 512:(nh + 1) * 512],
                        op=ALU.add), reads=[RP[bk], Rh[tt]], writes=[Rh[tt]])
                K.act(lambda e, tt=tt, i2=i2: e.activation(out=hn_f[i2], in_=hh[:, tt, :], func=AF.Square,
                                                           accum_out=ssE[i2]),
                      reads=[Rh[tt]], writes=[Rhn_f[i2], RssE[i2]])
                rms_rstd(ssE[i2], rsE[i2], RssE[i2], RrsE[i2], 1024)
                K.act(lambda e, tt=tt, i2=i2: e.activation(out=hn_f[i2], in_=hh[:, tt, :], func=AF.Copy,
                                                           scale=rsE[i2][:, 0:1]),
                      reads=[Rh[tt], RrsE[i2]], writes=[Rhn_f[i2]])
                for c in range(8):
                    bk = 2 + c // 4
                    K.pe(lambda e, bk=bk, c=c, i2=i2: e.transpose(out=psb[bk][:, (c % 4) * 128:(c % 4 + 1) * 128],
                                                                  in_=hn_f[i2][:, c * 128:(c + 1) * 128],
                                                                  identity=ident_f),
                         reads=[Rhn_f[i2], RG["ident_f"]], writes=[RP[bk]])
                for hf in range(2):
                    K.dve(lambda e, hf=hf, i2=i2: e.tensor_tensor(
                        out=hnT_f[i2][:, hf * 4:hf * 4 + 4, :], in0=psb[2 + hf][:, :].rearrange("p (c t) -> p c t", c=4),
                        in1=_bl(gcol_ffn[:, hf * 4:hf * 4 + 4], 128), op=ALU.mult),
                        reads=[RP[2 + hf], RG["gcol_ffn"]], writes=[RhnT_f[i2]])
                K.pool(lambda e, tt=tt, i2=i2: e.tensor_copy(out=hnT[:, :, tt * 128:(tt + 1) * 128], in_=hnT_f[i2]),
                       reads=[RhnT_f[i2]], writes=[RhnT[tt // 4]])
                for c in range(8):
                    K.pe(lambda e, c=c, i2=i2: e.matmul(psb[4 + i2][:, 0:36], lhsT=hnT_f[i2][:, c, :], rhs=wr[:, c, :],
                                                        start=(c == 0), stop=(c == 7)),
                         reads=[RhnT_f[i2], RG["wr"]], writes=[RP[4 + i2]])
                K.dve(lambda e, i2=i2, tt=tt: e.tensor_tensor(out=lgA[:, tt, :], in0=psb[4 + i2][:, 0:36], in1=rb_b, op=ALU.add),
                      reads=[RP[4 + i2], RG["rb_b"]], writes=[RlgA])

            lg4 = lgA[:, :, 0:4]
            elv = lgA[:, :, 4:36]
            K.dve(lambda e: e.tensor_reduce(out=gm, in_=lg4, axis=AX.X, op=ALU.max), reads=[RlgA], writes=[Rrt2])
            K.dve(lambda e: e.tensor_tensor(out=ex4, in0=lg4, in1=_bl(gm, 4), op=ALU.subtract), reads=[RlgA, Rrt2], writes=[Rex4])
            K.act(lambda e: e.activation(out=ex4, in_=ex4, func=AF.Exp), reads=[Rex4], writes=[Rex4])
            K.dve(lambda e: e.tensor_reduce(out=pg, in_=ex4, axis=AX.X, op=ALU.add), reads=[Rex4], writes=[Rrt2])
            K.dve(lambda e: e.reciprocal(out=pg, in_=pg), reads=[Rrt2], writes=[Rrt2])
            K.dve(lambda e: e.tensor_tensor(out=pen, in0=lg4, in1=_bl(gm, 4), op=ALU.is_ge), reads=[RlgA, Rrt2], writes=[Rpen])
            K.dve(lambda e: e.tensor_scalar(out=pen, in0=pen, scalar1=1e9, scalar2=-1e9, op0=ALU.mult, op1=ALU.add),
                  reads=[Rpen], writes=[Rpen])
            K.dve(lambda e: e.tensor_tensor(out=elA.rearrange("p t (g e) -> p t g e", g=4),
                                            in0=elv.rearrange("p t (g e) -> p t g e", g=4), in1=_bl(pen, 8), op=ALU.add),
                  reads=[RlgA, Rpen], writes=[RelA])
            K.dve(lambda e: e.tensor_reduce(out=m1, in_=elA, axis=AX.X, op=ALU.max), reads=[RelA], writes=[Rrt2])
            K.dve(lambda e: e.tensor_tensor(out=oh1, in0=elA, in1=_bl(m1, 32), op=ALU.is_ge), reads=[RelA, Rrt2], writes=[Roh1])
            K.dve(lambda e: e.scalar_tensor_tensor(out=elA, in0=oh1, scalar=-1e9, in1=elA, op0=ALU.mult, op1=ALU.add),
                  reads=[Roh1, RelA], writes=[RelA])
            K.dve(lambda e: e.tensor_reduce(out=m2, in_=elA, axis=AX.X, op=ALU.max), reads=[RelA], writes=[Rrt2])
            K.dve(lambda e: e.tensor_tensor(out=oh2, in0=elA, in1=_bl(m2, 32), op=ALU.is_ge), reads=[RelA, Rrt2], writes=[Roh2])
            K.dve(lambda e: e.tensor_tensor(out=dd, in0=m2, in1=m1, op=ALU.subtract), reads=[Rrt2], writes=[Rrt2])
            K.act(lambda e: e.activation(out=dd, in_=dd, func=AF.Exp), reads=[Rrt2], writes=[Rrt2])
            K.dve(lambda e: e.tensor_scalar(out=dd, in0=dd, scalar1=1.0, scalar2=None, op0=ALU.add), reads=[Rrt2], writes=[Rrt2])
            K.dve(lambda e: e.reciprocal(out=w1p, in_=dd), reads=[Rrt2], writes=[Rrt2])
            K.dve(lambda e: e.tensor_tensor(out=w1p, in0=w1p, in1=pg, op=ALU.mult), reads=[Rrt2], writes=[Rrt2])
            K.dve(lambda e: e.tensor_tensor(out=w2p, in0=pg, in1=w1p, op=ALU.subtract), reads=[Rrt2], writes=[Rrt2])
            K.dve(lambda e: e.tensor_tensor(out=comb, in0=oh1, in1=_bl(w1p, 32), op=ALU.mult), reads=[Roh1, Rrt2], writes=Rcomb)
            K.dve(lambda e: e.tensor_tensor(out=oh2, in0=oh2, in1=_bl(w2p, 32), op=ALU.mult), reads=[Roh2, Rrt2], writes=[Roh2])
            K.dve(lambda e: e.tensor_tensor(out=comb, in0=comb, in1=oh2, op=ALU.add), reads=[Roh2] + Rcomb, writes=Rcomb)

            if debug and b == 0:
                K.dma("sp", lambda e: e.dma_start(out=dbg_h.rearrange("(t p) d -> p t d", p=128), in_=hh), reads=Rh)
                K.dma("sp", lambda e: e.dma_start(out=dbg_comb.rearrange("(t p) d -> p t d", p=128), in_=comb), reads=Rcomb)
                K.dma("sp", lambda e: e.dma_start(out=dbg_hn.rearrange("(c p) t -> p c t", p=128), in_=hnT), reads=RhnT)
            if stop_after == 'E':
                break
            barrier()
            FA = Arena(R3.lo, R3.lo + 8192)
            Wgu = [FA.bf16(2, 8, 512) for _ in range(2)]
            R3.p = markE
            hid = R3.bf16(4, L)
            Rhid = RL(4)
            Wd = [R3.bf16(4, 1024) for _ in range(2)]
            sg = [R3.f32(512) for _ in range(2)]
            RWgu, RWd, Rsg = RL(2), RL(2), RL(2)
            it = 0
            for ex in range(NEXP):
                wi = ex % 2
                K.dma("pool", lambda e, wi=wi, ex=ex: e.dma_start(out=Wgu[wi][:, 0, :, :],
                                                                  in_=w_gate[0, ex].rearrange("(c p) n -> p c n", p=128)),
                      writes=[RWgu[wi]])
                K.dma("pool", lambda e, wi=wi, ex=ex: e.dma_start(out=Wgu[wi][:, 1, :, :],
                                                                  in_=w_up[0, ex].rearrange("(c p) n -> p c n", p=128)),
                      writes=[RWgu[wi]])
                K.dma("pool", lambda e, wi=wi, ex=ex: e.dma_start(out=Wd[wi], in_=w_down[0, ex].rearrange("(c p) n -> p c n", p=128)),
                      writes=[RWd[wi]])
                for tb in range(4):
                    for fc in range(4):
                        bg, bu = (it % 2) * 2, (it % 2) * 2 + 1
                        si = it % 2
                        it += 1
                        for gu, bk in ((0, bg), (1, bu)):
                            for c in range(8):
                                K.pe(lambda e, gu=gu, bk=bk, c=c, wi=wi, fc=fc, tb=tb: e.matmul(
                                    psb[bk][:, :], lhsT=Wgu[wi][:, gu, c, fc * 128:(fc + 1) * 128],
                                    rhs=hnT[:, c, tb * 512:(tb + 1) * 512], start=(c == 0), stop=(c == 7)),
                                    reads=[RWgu[wi], RhnT[tb]], writes=[RP[bk]])
                        K.act(lambda e, bg=bg, si=si: e.activation(out=sg[si], in_=psb[bg][:, :], func=AF.Silu),
                              reads=[RP[bg]], writes=[Rsg[si]])
                        K.dve(lambda e, bu=bu, si=si, fc=fc, tb=tb: e.tensor_tensor(
                            out=hid[:, fc, tb * 512:(tb + 1) * 512], in0=psb[bu][:, :], in1=sg[si], op=ALU.mult),
                            reads=[RP[bu], Rsg[si]], writes=[Rhid[tb]])
                for tt in range(NT):
                    for nh in range(2):
                        bk = 4 + (tt * 2 + nh) % 4
                        for fc in range(4):
                            K.pe(lambda e, bk=bk, fc=fc, tt=tt, nh=nh, wi=wi: e.matmul(
                                psb[bk][:, :], lhsT=hid[:, fc, tt * 128:(tt + 1) * 128],
                                rhs=Wd[wi][:, fc, nh * 512:(nh + 1) * 512], start=(fc == 0), stop=(fc == 3)),
                                reads=[Rhid[tt // 4], RWd[wi]], writes=[RP[bk]])
                        K.dve(lambda e, bk=bk, tt=tt, nh=nh, ex=ex: e.scalar_tensor_tensor(
                            out=hh[:, tt, nh * 512:(nh + 1) * 512], in0=psb[bk][:, :], scalar=comb[:, tt, ex:ex + 1],
                            in1=hh[:, tt, nh * 512:(nh + 1) * 512], op0=ALU.mult, op1=ALU.add),
                            reads=[RP[bk], Rcomb[tt], Rh[tt]], writes=[Rh[tt]])

            if stop_after == 'F':
                break
            ot = [R3.f32(1024) for _ in range(2)]
            ssG = [R3.f32(1) for _ in range(2)]
            rsG = [R3.f32(1) for _ in range(2)]
            sqG = R3.bf16(1024)
            Rot, RssG, RrsG, RsqG = RL(2), RL(2), RL(2), Res()
            for tt in range(NT):
                i2 = tt % 2
                K.act(lambda e, tt=tt, i2=i2: e.activation(out=sqG, in_=hh[:, tt, :], func=AF.Square, accum_out=ssG[i2]),
                      reads=[Rh[tt]], writes=[RsqG, RssG[i2]])
                rms_rstd(ssG[i2], rsG[i2], RssG[i2], RrsG[i2], 1024)
                K.dve(lambda e, tt=tt, i2=i2: e.scalar_tensor_tensor(out=ot[i2], in0=hh[:, tt, :], scalar=rsG[i2][:, 0:1],
                                                                     in1=fin_b, op0=ALU.mult, op1=ALU.mult),
                      reads=[Rh[tt], RrsG[i2], RG["fin_b"]], writes=[Rot[i2]])
                K.dma("sp", lambda e, tt=tt, i2=i2, b=b: e.dma_start(out=y[b, tt * 128:(tt + 1) * 128, :], in_=ot[i2]),
                      reads=[Rot[i2]])
            barrier()

        K.emit(block, sems, dsems)
    return nc


_NC_CACHE = {}


def _run(inputs, NB, NEXP=32, full=False):
    ncores = 8
    if NB not in _NC_CACHE:
        _NC_CACHE[NB] = build_nc(NB)
    nc = _NC_CACHE[NB]
    x = np.asarray(inputs["x"], dtype=np.float32)
    per = x.shape[0] // ncores
    oh = _onehot_tables()
    pow2 = (2.0 ** -np.arange(32, dtype=np.float64)).astype(np.float32)[None, :]
    shared = {k: np.ascontiguousarray(np.asarray(v, dtype=np.float32)) for k, v in inputs.items() if k != "x"}
    if NEXP != 32:
        for k in ("w_gate", "w_up", "w_down"):
            shared[k] = np.ascontiguousarray(shared[k][:, :NEXP])
    shared["ohtab"] = oh
    shared["pow2"] = pow2
    in_maps = []
    for c in range(ncores):
        m = dict(shared)
        m["x"] = np.ascontiguousarray(x[c * per:c * per + NB])
        in_maps.append(m)
    res = run_bass_kernel_spmd(nc, in_maps, core_ids=list(range(ncores)))
    if full:
        return res.results
    return [r["y"] for r in res.results]


def kernel(**inputs):
    outs = _run(inputs, 4)
    return np.concatenate(outs, axis=0).astype(np.float32)
```

```python
import math
import numpy as np
import concourse.bass as bass
import concourse.mybir as mybir
from concourse.ap import AP
from concourse.bass_utils import run_bass_kernel_spmd

F32 = mybir.dt.float32
BF16 = mybir.dt.bfloat16
AF = mybir.ActivationFunctionType
ALU = mybir.AluOpType
AX = mybir.AxisListType

ENGS = ("pe", "act", "dve", "pool", "sp")

L = 2048
D = 1024
NT = 16
INC = 5444
C_QA, C_KV, C_QI, C_KI, C_WI, C_DIL, C_G = 0, 512, 768, 1024, 1088, 1092, 3396
NEGM = -30000.0
TOPK = 256
NBIS = 14
DIL = ((128, 1), (512, 4), (2048, 16))
STREAMS = (("dsa", 0, 8, 2175, 2048), ("g0", 8, 4, 383, 256), ("g1", 12, 4, 767, 640), ("g2", 16, 4, 2175, 2048))
FROW = 2176


class Res:
    __slots__ = ("w", "r")

    def __init__(self):
        self.w = None
        self.r = []


def RL(n):
    return [Res() for _ in range(n)]


class Op:
    __slots__ = ("eng", "fn", "deps", "is_dma", "signal", "sigval", "dsem", "dval", "prev_dma")

    def __init__(self, eng, fn, is_dma):
        self.eng = eng
        self.fn = fn
        self.deps = ()
        self.is_dma = is_dma
        self.signal = False
        self.sigval = 0
        self.dsem = None
        self.dval = 0
        self.prev_dma = None


class Ker:
    def __init__(self):
        self.ops = []

    def _rec(self, eng, fn, reads, writes, is_dma=False):
        op = Op(eng, fn, is_dma)
        i = len(self.ops)
        deps = set()
        for r in reads:
            if r.w is not None:
                deps.add(r.w)
        for w in writes:
            if w.w is not None:
                deps.add(w.w)
            deps.update(w.r)
        op.deps = tuple(deps)
        for r in reads:
            r.r.append(i)
        for w in writes:
            w.w = i
            w.r = []
        self.ops.append(op)
        return i

    def pe(self, fn, reads=(), writes=()):
        return self._rec("pe", fn, reads, writes)

    def act(self, fn, reads=(), writes=()):
        return self._rec("act", fn, reads, writes)

    def dve(self, fn, reads=(), writes=()):
        return self._rec("dve", fn, reads, writes)

    def pool(self, fn, reads=(), writes=()):
        return self._rec("pool", fn, reads, writes)

    def dma(self, q, fn, reads=(), writes=()):
        return self._rec(q, fn, reads, writes, is_dma=True)

    def emit(self, block, sems, dma_sems):
        ops = self.ops
        for op in ops:
            for d in op.deps:
                dop = ops[d]
                if dop.is_dma:
                    continue
                if dop.eng == "pe" and op.eng == "pe" and not op.is_dma:
                    continue
                dop.signal = True
        cnt = {e: 0 for e in ENGS}
        dcount = {}
        dlast = {}
        dn = {e: 0 for e in ENGS}
        for op in ops:
            if op.is_dma:
                pool = dma_sems[op.eng]
                k = dn[op.eng] % len(pool)
                dn[op.eng] += 1
                key = (op.eng, k)
                dcount[key] = dcount.get(key, 0) + 1
                op.dsem = key
                op.dval = 16 * dcount[key]
                op.prev_dma = dlast.get(key)
                dlast[key] = op
            elif op.signal:
                cnt[op.eng] += 1
                op.sigval = cnt[op.eng]
        per_eng = {e: [] for e in ENGS}
        for op in ops:
            per_eng[op.eng].append(op)

        def semof(key):
            if isinstance(key, tuple):
                return dma_sems[key[0]][key[1]]
            return sems[key]

        def run_engine(ename, engine):
            known = {}
            for op in per_eng[ename]:
                need = {}
                for d in op.deps:
                    dop = ops[d]
                    if dop.is_dma:
                        key, val = dop.dsem, dop.dval
                    else:
                        if dop.eng == "pe" and ename == "pe" and not op.is_dma:
                            continue
                        key, val = dop.eng, dop.sigval
                    if need.get(key, 0) < val:
                        need[key] = val
                if op.is_dma and op.prev_dma is not None:
                    key, val = op.prev_dma.dsem, op.prev_dma.dval
                    if need.get(key, 0) < val:
                        need[key] = val
                for key, val in need.items():
                    if known.get(key, 0) >= val:
                        continue
                    engine.wait_ge(semof(key), val)
                    known[key] = val
                inst = op.fn(engine)
                if op.is_dma:
                    inst.then_inc(semof(op.dsem), 16)
                elif op.signal:
                    inst.then_inc(sems[ename], 1)
            for key, dop in dlast.items():
                if key[0] == ename and known.get(key, 0) < dop.dval:
                    engine.wait_ge(semof(key), dop.dval)

        @block.tensor
        def _(e):
            run_engine("pe", e)

        @block.scalar
        def _(e):
            run_engine("act", e)

        @block.vector
        def _(e):
            run_engine("dve", e)

        @block.gpsimd
        def _(e):
            run_engine("pool", e)

        @block.sync
        def _(e):
            run_engine("sp", e)


def _bucket(d):
    n = np.maximum(d, 0)
    nf = np.maximum(n, 1).astype(np.float32)
    large = 16 + (np.log(nf / np.float32(16)) / np.float32(math.log(2048 / 16)) * np.float32(16)).astype(np.int32)
    large = np.minimum(large, 31)
    return np.where(n < 16, n, large)


def _onehot_tables():
    cols = []
    for name, h0, nh, flen, ncols in STREAMS:
        d = np.arange(flen) - 127
        if name == "dsa":
            valid = d >= 0
        else:
            W, r = DIL[int(name[1])]
            valid = (d >= 0) & (d <= W) & (d % r == 0)
        oh = np.zeros((33, flen), np.float32)
        b = _bucket(d)
        for i in range(flen):
            if valid[i]:
                oh[b[i], i] = 1.0
            else:
                oh[32, i] = 1.0
        cols.append(oh)
    return np.concatenate(cols, axis=1)


def _bl(a, n):
    return AP(a.tensor, a.offset, [list(x) for x in a.ap] + [[0, n]])


def _bmid(a, n):
    ap = [list(x) for x in a.ap]
    return AP(a.tensor, a.offset, [ap[0], [0, n]] + ap[1:])


def build_nc(NB, stop_after=None, NEXP=32, NQT=16, NOATT=False, debug=False):
    nc = bass.Bass("TRN2", target_bir_lowering=False)

    def din(name, shape, dt=F32):
        return nc.dram_tensor(name, list(shape), dt, kind="ExternalInput").ap()

    x = din("x", [NB, L, D])
    attn_norm = din("attn_norm", [1, D])
    w_in = din("w_in", [1, D, INC])
    kv_norm = din("kv_norm", [1, 256])
    w_uk = din("w_uk", [1, 8, 256, 64])
    w_uv = din("w_uv", [1, 8, 256, 64])
    rel_bias = din("rel_bias", [32, 20])
    w_branch_a = din("w_branch_a", [1, 512, D])
    w_branch_b = din("w_branch_b", [1, 256, D])
    w_out = din("w_out", [1, D, D])
    ffn_norm = din("ffn_norm", [1, D])
    w_rg = din("w_router_group", [1, D, 4])
    b_rg = din("b_router_group", [1, 4])
    w_re = din("w_router_expert", [1, 4, D, 8])
    b_re = din("b_router_expert", [1, 4, 8])
    w_gate = din("w_gate", [1, NEXP, D, 512])
    w_up = din("w_up", [1, NEXP, D, 512])
    w_down = din("w_down", [1, NEXP, 512, D])
    final_norm = din("final_norm", [D])
    ohtab = din("ohtab", [33, 5500])
    pow2 = din("pow2", [1, 32])
    y = nc.dram_tensor("y", [NB, L, D], F32, kind="ExternalOutput").ap()
    fd = nc.dram_tensor("fd_scratch", [20, FROW], BF16, kind="Internal").ap()
    if debug:
        dbg_h = nc.dram_tensor("dbg_h", [L, D], F32, kind="ExternalOutput").ap()
        dbg_comb = nc.dram_tensor("dbg_comb", [L, 32], F32, kind="ExternalOutput").ap()
        dbg_ya = nc.dram_tensor("dbg_ya", [512, L], BF16, kind="ExternalOutput").ap()
        dbg_yb = nc.dram_tensor("dbg_yb", [256, L], BF16, kind="ExternalOutput").ap()
        dbg_mix = nc.dram_tensor("dbg_mix", [1024, L], BF16, kind="ExternalOutput").ap()
        dbg_hn = nc.dram_tensor("dbg_hn", [1024, L], BF16, kind="ExternalOutput").ap()
        dbg_cT = nc.dram_tensor("dbg_cT", [128, 2 * L], BF16, kind="ExternalOutput").ap()
        dbg_vh = nc.dram_tensor("dbg_vh", [128, 16 * 8 * 65], BF16, kind="ExternalOutput").ap()
        dbg_kh = nc.dram_tensor("dbg_kh", [128, 4 * L], BF16, kind="ExternalOutput").ap()
        dbg_vd = nc.dram_tensor("dbg_vd", [128, 8 * 2048], BF16, kind="ExternalOutput").ap()
        dbg_qa = nc.dram_tensor("dbg_qa", [128, 4 * L], BF16, kind="ExternalOutput").ap()
        dbg_yaf = nc.dram_tensor("dbg_yaf", [128, 8 * 65], F32, kind="ExternalOutput").ap()
        dbg_p0 = nc.dram_tensor("dbg_p0", [128, 512], BF16, kind="ExternalOutput").ap()
        dbg_p1 = nc.dram_tensor("dbg_p1", [128, 512], BF16, kind="ExternalOutput").ap()
        dbg_qp = nc.dram_tensor("dbg_qp", [128, 1024], BF16, kind="ExternalOutput").ap()

    K = Ker()
    from contextlib import ExitStack
    with ExitStack() as es:
        ARENA_W = 52400
        arena = es.enter_context(nc.sbuf_tensor("arena", [128, ARENA_W], F32))
        psb = [es.enter_context(nc.psum_tensor(f"psb{i}", [128, 512], F32)) for i in range(8)]
        RP = RL(8)
        sems = {e: es.enter_context(nc.semaphore("s_" + e)) for e in ENGS}
        dsems = {e: [es.enter_context(nc.semaphore(f"d_{e}{i}")) for i in range(8)] for e in ("sp", "pool", "act")}
        dsems["pe"] = []
        dsems["dve"] = []
        block = es.enter_context(nc.Block())

        class Arena:
            def __init__(self, lo, hi):
                self.lo, self.hi, self.p = lo, hi, lo

            def f32(self, *shape):
                n = int(np.prod(shape))
                off = self.p
                self.p += n + (n & 1)
                assert self.p <= self.hi, ("arena overflow", self.p, self.hi)
                v = arena[:, off:off + n]
                return self._shape(v, shape)

            def bf16(self, *shape):
                n = int(np.prod(shape))
                w = (n + 1) // 2
                w += (w & 1)
                off = self.p
                self.p += w
                assert self.p <= self.hi, ("arena overflow", self.p, self.hi)
                v = arena[:, off:off + w].bitcast(BF16)[:, 0:n]
                return self._shape(v, shape)

            @staticmethod
            def _shape(v, shape):
                if len(shape) == 1:
                    return v
                if len(shape) == 2:
                    return v.rearrange("p (a b) -> p a b", a=shape[0])
                if len(shape) == 3:
                    return v.rearrange("p (a b c) -> p a b c", a=shape[0], b=shape[1])
                raise ValueError

            def reset(self):
                self.p = self.lo

        G = Arena(0, 4200)
        ident_f = G.f32(128)
        ident_b = G.bf16(128)
        J_b = G.bf16(128)
        zero_b = G.bf16(512)
        tmp_f = G.f32(128)
        gcol_attn = G.f32(8)
        gcol_ffn = G.f32(8)
        kvn_b = G.f32(256)
        fin_b = G.f32(1024)
        wr = G.f32(8, 36)
        rb_b = G.f32(36)
        wuk = G.bf16(2, 512)
        wuv = G.bf16(2, 512)
        tabA = G.f32(20)
        pow2_b = G.f32(32)
        tri = G.f32(128)
        sgb = G.f32(32)
        R1 = Arena(G.hi, G.hi + 16384)
        R3 = Arena(R1.hi, ARENA_W)
        RG = {n: Res() for n in ("ident_f", "ident_b", "J_b", "zero_b", "tmp_f", "gcol_attn", "gcol_ffn", "kvn_b",
                                 "fin_b", "wr", "rb_b", "wuk", "wuv", "tabA", "pow2_b", "fd", "tri", "sgb")}

        def barrier():
            last = {}
            dmas = {}
            for i, op in enumerate(K.ops):
                if op.is_dma:
                    dmas.setdefault(op.eng, []).append(i)
                else:
                    last[op.eng] = i
            deps = tuple(last.values())
            for q, lst in dmas.items():
                deps = deps + tuple(lst[-8:])
            for eng in ("pe", "act", "dve", "pool", "sp"):
                if eng == "pe":
                    continue
                op = Op(eng, (lambda e: e.nop()), False)
                op.deps = deps
                K.ops.append(op)
            op = Op("pe", (lambda e: e.nop()), False)
            op.deps = deps
            K.ops.append(op)

        K.pool(lambda e: e.memset(ident_f, 1.0), writes=[RG["ident_f"]])
        K.pool(lambda e: e.affine_select(out=ident_f, in_=ident_f, pattern=[[1, 128]], compare_op=ALU.is_equal,
                                         fill=0.0, base=0, channel_multiplier=-1),
               reads=[RG["ident_f"]], writes=[RG["ident_f"]])
        K.dve(lambda e: e.tensor_copy(out=ident_b, in_=ident_f), reads=[RG["ident_f"]], writes=[RG["ident_b"]])
        K.pool(lambda e: e.memset(tmp_f, 1.0), writes=[RG["tmp_f"]])
        K.pool(lambda e: e.affine_select(out=tmp_f, in_=tmp_f, pattern=[[1, 128]], compare_op=ALU.is_equal,
                                         fill=0.0, base=-127, channel_multiplier=1),
               reads=[RG["tmp_f"]], writes=[RG["tmp_f"]])
        K.dve(lambda e: e.tensor_copy(out=J_b, in_=tmp_f), reads=[RG["tmp_f"]], writes=[RG["J_b"]])
        K.pool(lambda e: e.memset(zero_b, 0.0), writes=[RG["zero_b"]])
        K.pool(lambda e: e.memset(tri, 0.0), writes=[RG["tri"]])
        for i_ in range(1, 17):
            K.pool(lambda e, i_=i_: e.memset(sgb[:, i_:i_ + 1], float(i_ * 128) - 511.5), writes=[RG["sgb"]])
        K.pool(lambda e: e.affine_select(out=tri, in_=tri, pattern=[[-1, 128]], compare_op=ALU.is_ge,
                                         fill=-1e30, base=0, channel_multiplier=1),
               reads=[RG["tri"]], writes=[RG["tri"]])
        K.dma("sp", lambda e: e.dma_start(out=gcol_attn, in_=attn_norm[0].rearrange("(c p) -> p c", p=128),
                                          allow_slow_non_contiguous=True), writes=[RG["gcol_attn"]])
        K.dma("sp", lambda e: e.dma_start(out=gcol_ffn, in_=ffn_norm[0].rearrange("(c p) -> p c", p=128),
                                          allow_slow_non_contiguous=True), writes=[RG["gcol_ffn"]])
        K.dma("sp", lambda e: e.dma_start(out=kvn_b, in_=AP(kv_norm.tensor, 0, [[0, 128], [1, 256]])),
              writes=[RG["kvn_b"]])
        K.dma("sp", lambda e: e.dma_start(out=fin_b, in_=AP(final_norm.tensor, 0, [[0, 128], [1, 1024]])),
              writes=[RG["fin_b"]])
        K.dma("sp", lambda e: e.dma_start(out=pow2_b, in_=AP(pow2.tensor, 0, [[0, 128], [1, 32]])),
              writes=[RG["pow2_b"]])
        K.dma("sp", lambda e: e.dma_start(out=wr[:, :, 0:4], in_=w_rg[0].rearrange("(c p) n -> p c n", p=128)),
              writes=[RG["wr"]])
        for g in range(4):
            K.dma("sp", lambda e, g=g: e.dma_start(out=wr[:, :, 4 + 8 * g:12 + 8 * g],
                                                   in_=w_re[0, g].rearrange("(c p) n -> p c n", p=128)),
                  writes=[RG["wr"]])
        K.dma("sp", lambda e: e.dma_start(out=rb_b[:, 0:4], in_=AP(b_rg.tensor, 0, [[0, 128], [1, 4]])),
              writes=[RG["rb_b"]])
        K.dma("sp", lambda e: e.dma_start(out=rb_b[:, 4:36], in_=AP(b_re.tensor, 0, [[0, 128], [1, 32]])),
              writes=[RG["rb_b"]])
        for cc in range(2):
            K.dma("pool", lambda e, cc=cc: e.dma_start(out=wuk[:, cc, :].rearrange("p (h d) -> p h d", h=8),
                                                       in_=w_uk[0, :, cc * 128:(cc + 1) * 128, :].rearrange("h p d -> p h d")),
                  writes=[RG["wuk"]])
            K.dma("pool", lambda e, cc=cc: e.dma_start(out=wuv[:, cc, :].rearrange("p (h d) -> p h d", h=8),
                                                       in_=w_uv[0, :, cc * 128:(cc + 1) * 128, :].rearrange("h p d -> p h d")),
                  writes=[RG["wuv"]])
        K.dma("sp", lambda e: e.dma_start(out=tabA[0:32, :], in_=rel_bias[:, :]), writes=[RG["tabA"]])
        K.pool(lambda e: e.memset(tabA[32:33, :], NEGM), writes=[RG["tabA"]])

        R3.reset()
        ohs = R3.f32(5500)
        fsb = R3.bf16(FROW)
        Rohs, Rfsb = Res(), Res()
        K.dma("sp", lambda e: e.dma_start(out=ohs[0:33, :], in_=ohtab[:, :]), writes=[Rohs])
        col0 = 0
        pi = 0
        for name, h0, nh, flen, ncols in STREAMS:
            for c0 in range(0, flen, 512):
                n = min(512, flen - c0)
                bk = pi % 2
                pi += 1
                K.pe(lambda e, bk=bk, h0=h0, nh=nh, a=col0 + c0, n=n: e.matmul(
                    psb[bk][0:nh, 0:n], lhsT=tabA[0:33, h0:h0 + nh], rhs=ohs[0:33, a:a + n], start=True, stop=True),
                    reads=[RG["tabA"], Rohs], writes=[RP[bk]])
                K.dve(lambda e, bk=bk, nh=nh, c0=c0, n=n: e.tensor_copy(out=fsb[0:nh, c0:c0 + n], in_=psb[bk][0:nh, 0:n]),
                      reads=[RP[bk]], writes=[Rfsb])
            K.dma("sp", lambda e, h0=h0, nh=nh, flen=flen: e.dma_start(out=fd[h0:h0 + nh, 0:flen], in_=fsb[0:nh, 0:flen]),
                  reads=[Rfsb], writes=[RG["fd"]])
            col0 += flen
        barrier()

        rot = {"ps": 0}

        def build_V(V, RV, si, stage, Rstage, banks):
            name, h0, nh, flen, ncols = STREAMS[si]
            for hl in range(nh):
                sb_i = hl % 2
                K.dma("sp", lambda e, sb_i=sb_i, hl=hl: e.dma_start(
                    out=stage[sb_i][:, 0:ncols], in_=AP(fd.tensor, (h0 + hl) * FROW, [[1, 128], [1, ncols]])),
                    reads=[RG["fd"]], writes=[Rstage[sb_i]])
                for c0 in range(0, ncols, 512):
                    n = min(512, ncols - c0)
                    bk = banks[rot["ps"] % len(banks)]
                    rot["ps"] += 1
                    K.pe(lambda e, bk=bk, sb_i=sb_i, c0=c0, n=n: e.matmul(
                        psb[bk][:, 0:n], lhsT=J_b, rhs=stage[sb_i][:, c0:c0 + n], start=True, stop=True),
                        reads=[RG["J_b"], Rstage[sb_i]], writes=[RP[bk]])
                    K.act(lambda e, bk=bk, hl=hl, c0=c0, n=n: e.copy(out=V[:, hl, c0:c0 + n], in_=psb[bk][:, 0:n]),
                          reads=[RP[bk]], writes=[RV])

        evt = {"i": 0}

        def evac_copy(out, in_, reads, writes, scale=None, force=None):
            evt["i"] += 1
            if (evt["i"] % 2 == 0 and force is None) or force == "act":
                if scale is None:
                    K.act(lambda e: e.copy(out=out, in_=in_), reads=reads, writes=writes)
                else:
                    K.act(lambda e: e.mul(out=out, in_=in_, mul=scale), reads=reads, writes=writes)
            else:
                if scale is None:
                    K.dve(lambda e: e.tensor_copy(out=out, in_=in_), reads=reads, writes=writes)
                else:
                    K.dve(lambda e: e.tensor_scalar(out=out, in0=in_, scalar1=scale, scalar2=None, op0=ALU.mult),
                          reads=reads, writes=writes)

        def load_w(Wt, RW, col0, ncols, dst0=0):
            K.dma("pool", lambda e: e.dma_start(out=Wt[:, :, dst0:dst0 + ncols],
                                                in_=w_in[0, :, col0:col0 + ncols].rearrange("(c p) n -> p c n", p=128)),
                  writes=[RW])

        def proj_fm(Wt, RW, wcol0, M, uT, RuT, dst_fn, Rdst, banks, scale=None):
            for tb in range(4):
                bk = banks[rot["ps"] % len(banks)]
                rot["ps"] += 1
                for c in range(8):
                    K.pe(lambda e, bk=bk, c=c, tb=tb: e.matmul(
                        psb[bk][0:M, :], lhsT=Wt[:, c, wcol0:wcol0 + M], rhs=uT[:, c, tb * 512:(tb + 1) * 512],
                        start=(c == 0), stop=(c == 7)),
                        reads=[RW] + RuT[tb * 4:(tb + 1) * 4], writes=[RP[bk]])
                evac_copy(dst_fn(tb), psb[bk][0:M, :], [RP[bk]], [Rdst[tb]], scale)

        def rms_rstd(ss, rstd, Rss, Rrstd, n):
            K.dve(lambda e: e.tensor_scalar(out=rstd, in0=ss, scalar1=1.0 / n, scalar2=1e-6, op0=ALU.mult, op1=ALU.add),
                  reads=[Rss], writes=[Rrstd])
            K.act(lambda e: e.activation(out=rstd, in_=rstd, func=AF.Sqrt), reads=[Rrstd], writes=[Rrstd])
            K.dve(lambda e: e.reciprocal(out=rstd, in_=rstd), reads=[Rrstd], writes=[Rrstd])

        def attention(nheads, qT, RqT, kT, RkT, vv, Rvv, V, RV, dmax, qt, maskT, RmaskT, lg_banks, pv_banks,
                      finalize, psb_sb, Rpsb_sb, qpads):
            nhalf = nheads // 4
            qp, Rqp = qpads[qt % 2]
            nj = nheads // 2
            qpv = qp.rearrange("p (j two) t -> p j two t", two=2)
            K.pool(lambda e: e.tensor_copy(out=qpv[0:64, :, 0, :], in_=qT[0:64, 0:nj, qt * 128:(qt + 1) * 128]),
                   reads=[RqT[qt // 4]], writes=[Rqp])
            K.pool(lambda e: e.tensor_copy(out=qpv[64:128, :, 1, :], in_=qT[64:128, 0:nj, qt * 128:(qt + 1) * 128]),
                   reads=[RqT[qt // 4]], writes=[Rqp])
            for hf in range(nhalf):
                K.pe(lambda e, hf=hf: e.matmul(psb[pv_banks[hf]][:, 0:260], lhsT=zero_b[:, 0:128], rhs=zero_b[:, 0:260],
                                               start=True, stop=False, skip_group_check=True),
                     reads=[RG["zero_b"]], writes=[RP[pv_banks[hf]]])
            kts = list(range(max(0, qt - dmax), qt + 1))
            pend = None

            def do_pv(item):
                kt, pbufs = item
                for hf in range(nhalf):
                    for hh in range(4):
                        h = hf * 4 + hh
                        K.pe(lambda e, hf=hf, hh=hh, h=h, kt=kt, pb=pbufs[hf]: e.matmul(
                            psb[pv_banks[hf]][:, hh * 65:hh * 65 + 65], lhsT=psb_sb[pb][:, hh * 128:(hh + 1) * 128],
                            rhs=vv[:, kt, h, :], start=False, stop=(kt == qt), skip_group_check=True),
                            reads=[Rpsb_sb[pbufs[hf]], Rvv[kt]], writes=[RP[pv_banks[hf]]])

            for kt in kts:
                dl = qt - kt
                pbufs = []
                for hf in range(nhalf):
                    bk = lg_banks[rot["lg"] % len(lg_banks)]
                    pb = rot["lg"] % len(psb_sb)
                    rot["lg"] += 1
                    pbufs.append(pb)
                    K.pe(lambda e, bk=bk, hf=hf, dl=dl: e.matmul(
                        psb[bk][:, :].rearrange("p (h t) -> p h t", h=4), lhsT=ident_b,
                        rhs=V[:, hf * 4:hf * 4 + 4, dl * 128:(dl + 1) * 128], start=True, stop=False,
                        skip_group_check=True),
                        reads=[RG["ident_b"], RV], writes=[RP[bk]])
                    if maskT is not None:
                        K.pe(lambda e, bk=bk, kt=kt: e.matmul(
                            psb[bk][:, :].rearrange("p (h t) -> p h t", h=4), lhsT=ident_b,
                            rhs=_bmid(maskT[:, kt, :], 4), start=False, stop=False, skip_group_check=True),
                            reads=[RG["ident_b"], RmaskT], writes=[RP[bk]])
                    for hh in range(4):
                        h = hf * 4 + hh
                        j = h // 2
                        K.pe(lambda e, bk=bk, hh=hh, j=j, h=h, kt=kt: e.matmul(
                            psb[bk][:, hh * 128:(hh + 1) * 128], lhsT=kT[:, j, kt * 128:(kt + 1) * 128],
                            rhs=qp[:, h, :], start=False, stop=(hh == 3), skip_group_check=True),
                            reads=[RkT[kt // 4], Rqp], writes=[RP[bk]])
                    K.act(lambda e, bk=bk, pb=pb: e.activation(out=psb_sb[pb], in_=psb[bk][:, :], func=AF.Exp),
                          reads=[RP[bk]], writes=[Rpsb_sb[pb]])
                if pend is not None:
                    do_pv(pend)
                pend = (kt, pbufs)
            do_pv(pend)
            finalize()

        rot["lg"] = 0

        for b in range(NB):
            if stop_after == 'S':
                break
            R1.reset()
            R3.reset()
            uT = R1.bf16(8, L)
            y_aT = R1.bf16(4, L)
            y_bT = R1.bf16(2, L)
            RuT = RL(16)
            Ry_aT = RL(4)
            Ry_bT = RL(4)
            xst = [R3.f32(1024) for _ in range(3)]
            xn = [R3.bf16(1024) for _ in range(2)]
            sqj = R3.bf16(1024)
            ssA = [R3.f32(1) for _ in range(3)]
            rsA = [R3.f32(1) for _ in range(3)]
            Rxst, Rxn, RssA, RrsA, Rsqj = RL(3), RL(2), RL(3), RL(3), Res()
            for tt in range(NT):
                i3, i2 = tt % 3, tt % 2
                K.dma("sp", lambda e, i3=i3, tt=tt, b=b: e.dma_start(out=xst[i3], in_=x[b, tt * 128:(tt + 1) * 128, :]),
                      writes=[Rxst[i3]])
                K.act(lambda e, i3=i3: e.activation(out=sqj, in_=xst[i3], func=AF.Square, accum_out=ssA[i3]),
                      reads=[Rxst[i3]], writes=[Rsqj, RssA[i3]])
                rms_rstd(ssA[i3], rsA[i3], RssA[i3], RrsA[i3], 1024)
                K.act(lambda e, i3=i3, i2=i2: e.activation(out=xn[i2], in_=xst[i3], func=AF.Copy, scale=rsA[i3][:, 0:1]),
                      reads=[Rxst[i3], RrsA[i3]], writes=[Rxn[i2]])
                bk = tt % 2
                pT = psb[bk][:, :].bitcast(BF16)[:, 0:1024].rearrange("p (c t) -> p c t", c=8)
                for c in range(8):
                    K.pe(lambda e, c=c, i2=i2, pT=pT: e.transpose(out=pT[:, c, :], in_=xn[i2][:, c * 128:(c + 1) * 128],
                                                                  identity=ident_b),
                         reads=[Rxn[i2], RG["ident_b"]], writes=[RP[bk]])
                K.dve(lambda e, pT=pT, tt=tt: e.tensor_tensor(out=uT[:, :, tt * 128:(tt + 1) * 128], in0=pT,
                                                              in1=_bl(gcol_attn, 128), op=ALU.mult),
                      reads=[RP[bk], RG["gcol_attn"]], writes=[RuT[tt]])
            barrier()

            if stop_after == 'A':
                break
            R3.reset()
            q_aT = R3.bf16(4, L)
            q_iT = R1.bf16(2, L)
            k_iT = y_bT
            k_hT = R3.bf16(4, L)
            v_h = R3.bf16(NT, 8, 65)
            Vd = R3.bf16(8, 2048)
            score = R3.f32(2048)
            junk = R3.bf16(2048)
            cT = score.bitcast(BF16).rearrange("p (c t) -> p c t", c=2)
            mask_b = R3.bf16(2048)
            maskT = [R3.bf16(NT, 128) for _ in range(2)]
            p_sb = [R3.bf16(512) for _ in range(4)]
            Wst = [m.rearrange("p a b -> p (a b)").rearrange("p (c n) -> p c n", c=8) for m in maskT]
            Vst = [junk, mask_b]
            c_tok = [R3.bf16(256) for _ in range(4)]
            rl = [R3.f32(512) for _ in range(2)]
            absw = R3.f32(NT, 4)
            sgn = R3.f32(NT, 4)
            ssB = [R3.f32(1) for _ in range(4)]
            rsB = [R3.f32(1) for _ in range(4)]
            sm = R3.f32(8)
            sB = R3.f32(1)
            junkB = junk
            RsB = Res()
            stepT = R3.f32(32)
            ya_f = R3.f32(8, 65)
            rec8 = R3.f32(8)
            ya_b = R3.bf16(512)
            qpads = [(R3.bf16(8, 128), Res()) for _ in range(2)]
            for qp_, Rqp_ in qpads:
                K.pool(lambda e, qp_=qp_: e.memset(qp_, 0.0), writes=[Rqp_])
            Rq_aT, Rq_iT, Rk_iT, Rk_hT = RL(4), RL(4), RL(4), RL(4)
            Rv_h = RL(NT)
            RVd, Rscore, Rjunk, Rmask_b = Res(), Res(), Res(), Res()
            RjunkB = Rjunk
            RcT = Rscore
            RmaskT, Rp_sb, Rc_tok, Rrl = RL(2), RL(4), RL(4), RL(2)
            RWst = RmaskT
            RVst = [Rjunk, Rmask_b]
            Rabsw, Rsgn, RssB, RrsB = Res(), Res(), RL(4), RL(4)
            Rsm, RstepT, Rya_f, Rrec8, Rya_b = Res(), Res(), Res(), Res(), Res()

            build_V(Vd, RVd, 0, Vst, RVst, [0, 1])
            if stop_after == 'B1':
                break
            K.pool(lambda e: e.memset(v_h[:, :, :, 64:65], 1.0), writes=Rv_h)
            for j in range(4):
                wi = j % 2
                load_w(Wst[wi], RWst[wi], C_QA + j * 128, 128)
                proj_fm(Wst[wi], RWst[wi], 0, 128, uT, RuT, lambda tb, j=j: q_aT[:, j, tb * 512:(tb + 1) * 512],
                        Rq_aT, [0, 1])
            for j in range(2):
                wi = j % 2
                load_w(Wst[wi], RWst[wi], C_QI + j * 128, 128)
                proj_fm(Wst[wi], RWst[wi], 0, 128, uT, RuT, lambda tb, j=j: q_iT[:, j, tb * 512:(tb + 1) * 512],
                        Rq_iT, [0, 1])
            for v in range(2):
                K.pool(lambda e, v=v: e.memset(Wst[v][:, :, 0:128], 0.0), writes=[RWst[v]])
                load_w(Wst[v], RWst[v], C_KI, 64, 64 * v)
                proj_fm(Wst[v], RWst[v], 0, 128, uT, RuT, lambda tb, v=v: k_iT[:, v, tb * 512:(tb + 1) * 512], Rk_iT, [0, 1])
            if stop_after == 'B2':
                break
            load_w(Wst[1], RWst[1], C_KV, 256, 0)
            Wx = R3.bf16(8, 4)
            RWx = Res()
            load_w(Wx, RWx, C_WI, 4, 0)
            for tt in range(NT):
                bk = (0, 1, 4, 5)[tt % 4]
                i2 = tt % 4
                for c in range(8):
                    K.pe(lambda e, bk=bk, c=c, tt=tt: e.matmul(psb[bk][:, 0:256], lhsT=uT[:, c, tt * 128:(tt + 1) * 128],
                                                               rhs=Wst[1][:, c, 0:256], start=(c == 0), stop=(c == 7)),
                         reads=[RuT[tt], RWst[1]], writes=[RP[bk]])
                for c in range(8):
                    K.pe(lambda e, bk=bk, c=c, tt=tt: e.matmul(psb[bk][:, 256:260], lhsT=uT[:, c, tt * 128:(tt + 1) * 128],
                                                               rhs=Wx[:, c, 0:4], start=(c == 0), stop=(c == 7),
                                                               skip_group_check=True),
                         reads=[RuT[tt], RWx], writes=[RP[bk]])
                K.act(lambda e, bk=bk, i2=i2: e.activation(out=junk[:, 0:256], in_=psb[bk][:, 0:256], func=AF.Square,
                                                           accum_out=ssB[i2]),
                      reads=[RP[bk]], writes=[Rjunk, RssB[i2]])
                rms_rstd(ssB[i2], rsB[i2], RssB[i2], RrsB[i2], 256)
                K.dve(lambda e, bk=bk, i2=i2: e.scalar_tensor_tensor(out=c_tok[i2], in0=psb[bk][:, 0:256],
                                                                     scalar=rsB[i2][:, 0:1], in1=kvn_b,
                                                                     op0=ALU.mult, op1=ALU.mult),
                      reads=[RP[bk], RrsB[i2], RG["kvn_b"]], writes=[Rc_tok[i2]])
                K.act(lambda e, bk=bk, tt=tt: e.activation(out=absw[:, tt, :], in_=psb[bk][:, 256:260], func=AF.Abs,
                                                           scale=0.0625),
                      reads=[RP[bk]], writes=[Rabsw])
                K.dve(lambda e, bk=bk, tt=tt: e.tensor_scalar(out=sgn[:, tt, :], in0=psb[bk][:, 256:260], scalar1=0.0,
                                                              scalar2=2.0, op0=ALU.is_gt, op1=ALU.mult),
                      reads=[RP[bk]], writes=[Rsgn])
                K.dve(lambda e, tt=tt: e.tensor_scalar(out=sgn[:, tt, :], in0=sgn[:, tt, :], scalar1=-1.0, scalar2=None,
                                                       op0=ALU.add),
                      reads=[Rsgn], writes=[Rsgn])
                bk2 = (2, 3, 6, 7)[tt % 4]
                pT2 = psb[bk2][:, :].bitcast(BF16)[:, 0:256].rearrange("p (c t) -> p c t", c=2)
                for cc in range(2):
                    K.pe(lambda e, cc=cc, i2=i2, pT2=pT2: e.transpose(out=pT2[:, cc, :],
                                                                      in_=c_tok[i2][:, cc * 128:(cc + 1) * 128],
                                                                      identity=ident_b),
                         reads=[Rc_tok[i2], RG["ident_b"]], writes=[RP[bk2]])
                evac_copy(cT[:, :, tt * 128:(tt + 1) * 128], pT2, [RP[bk2]], [RcT])
            if stop_after == 'B3':
                break
            for j in range(4):
                for kb in range(4):
                    bk = rot["ps"] % 2
                    rot["ps"] += 1
                    for cc in range(2):
                        K.pe(lambda e, bk=bk, cc=cc, j=j, kb=kb: e.matmul(
                            psb[bk][:, :], lhsT=wuk[:, cc, j * 128:(j + 1) * 128], rhs=cT[:, cc, kb * 512:(kb + 1) * 512],
                            start=(cc == 0), stop=(cc == 1)), reads=[RG["wuk"], RcT], writes=[RP[bk]])
                    evac_copy(k_hT[:, j, kb * 512:(kb + 1) * 512], psb[bk][:, :], [RP[bk]], [Rk_hT[kb]], 0.125)
            for tt in range(NT):
                bk = rot["ps"] % 2
                rot["ps"] += 1
                for cc in range(2):
                    K.pe(lambda e, bk=bk, cc=cc, tt=tt: e.matmul(
                        psb[bk][:, :], lhsT=cT[:, cc, tt * 128:(tt + 1) * 128], rhs=wuv[:, cc, :],
                        start=(cc == 0), stop=(cc == 1)), reads=[RG["wuv"], RcT], writes=[RP[bk]])
                evac_copy(v_h[:, tt, :, 0:64], psb[bk][:, :].rearrange("p (h d) -> p h d", h=8), [RP[bk]], [Rv_h[tt]])

            if debug and b == 0:
                K.dma("sp", lambda e: e.dma_start(out=dbg_cT, in_=cT.rearrange("p c t -> p (c t)")), reads=[RcT])
                K.dma("sp", lambda e: e.dma_start(out=dbg_vh, in_=v_h.rearrange("p a b c -> p (a b c)")), reads=Rv_h)
                K.dma("sp", lambda e: e.dma_start(out=dbg_kh, in_=k_hT.rearrange("p c t -> p (c t)")), reads=Rk_hT)
                K.dma("sp", lambda e: e.dma_start(out=dbg_vd, in_=Vd.rearrange("p c t -> p (c t)")), reads=[RVd])
                K.dma("sp", lambda e: e.dma_start(out=dbg_qa, in_=q_aT.rearrange("p c t -> p (c t)")), reads=Rq_aT)
            if stop_after == 'B4':
                break
            def stA1(qt):
                nk = (qt + 1) * 128
                if True:
                    for kb in range((nk + 511) // 512):
                        ncol = min(512, nk - kb * 512)
                        for h in range(4):
                            bk = rot["ps"] % 2
                            rot["ps"] += 1
                            j, pbase = h // 2, (h % 2) * 64
                            ri = rot["ps"] % 2
                            K.pe(lambda e, bk=bk, j=j, h=h, kb=kb, ncol=ncol, qt=qt: e.matmul(
                                psb[bk][:, 0:ncol], lhsT=q_iT[:, j, qt * 128:(qt + 1) * 128],
                                rhs=k_iT[:, h % 2, kb * 512:kb * 512 + ncol], start=True, stop=True),
                                reads=[Rq_iT[qt // 4], Rk_iT[kb]], writes=[RP[bk]])
                            K.act(lambda e, bk=bk, ri=ri, ncol=ncol, qt=qt, h=h: e.activation(
                                out=rl[ri][:, 0:ncol], in_=psb[bk][:, 0:ncol], func=AF.Relu, scale=absw[:, qt, h:h + 1]),
                                reads=[RP[bk], Rabsw], writes=[Rrl[ri]])
                            if h == 0:
                                K.dve(lambda e, ri=ri, kb=kb, ncol=ncol, qt=qt: e.tensor_scalar(
                                    out=score[:, kb * 512:kb * 512 + ncol], in0=rl[ri][:, 0:ncol],
                                    scalar1=sgn[:, qt, 0:1], scalar2=None, op0=ALU.mult),
                                    reads=[Rrl[ri], Rsgn], writes=[Rscore])
                            else:
                                K.dve(lambda e, ri=ri, kb=kb, ncol=ncol, qt=qt, h=h: e.scalar_tensor_tensor(
                                    out=score[:, kb * 512:kb * 512 + ncol], in0=rl[ri][:, 0:ncol],
                                    scalar=sgn[:, qt, h:h + 1], in1=score[:, kb * 512:kb * 512 + ncol],
                                    op0=ALU.mult, op1=ALU.add),
                                    reads=[Rrl[ri], Rsgn, Rscore], writes=[Rscore])
                    K.pool(lambda e, qt=qt: e.tensor_tensor(
                        out=score[:, qt * 128:(qt + 1) * 128], in0=score[:, qt * 128:(qt + 1) * 128], in1=tri, op=ALU.add),
                        reads=[Rscore, RG["tri"]], writes=[Rscore])
                    nc0 = qt * 128
                    K.dve(lambda e, nk=nk: e.tensor_reduce(out=sm[:, 0:1], in_=score[:, 0:nk], axis=AX.X, op=ALU.max),
                          reads=[Rscore], writes=[Rsm])
                    K.dve(lambda e: e.tensor_reduce(out=sm[:, 1:2], in_=score[:, 0:256], axis=AX.X, op=ALU.min),
                          reads=[Rscore], writes=[Rsm])
                    K.dve(lambda e: e.tensor_scalar(out=sm[:, 1:2], in0=sm[:, 1:2], scalar1=-1.0, scalar2=None, op0=ALU.add),
                          reads=[Rsm], writes=[Rsm])
                    K.dve(lambda e: e.tensor_tensor(out=sm[:, 2:3], in0=sm[:, 0:1], in1=sm[:, 1:2], op=ALU.subtract),
                          reads=[Rsm], writes=[Rsm])
                    K.dve(lambda e: e.tensor_scalar(out=stepT, in0=pow2_b, scalar1=sm[:, 2:3], scalar2=None, op0=ALU.mult),
                          reads=[Rsm, RG["pow2_b"]], writes=[RstepT])
                    K.dve(lambda e: e.tensor_tensor(out=sm[:, 5:6], in0=sm[:, 1:2], in1=stepT[:, 1:2], op=ALU.add),
                          reads=[Rsm, RstepT], writes=[Rsm])
                    for k in range(1, NBIS + 1):
                        K.dve(lambda e, nk=nk: e.tensor_scalar(out=junk[:, 0:nk], in0=score[:, 0:nk], scalar1=sm[:, 5:6],
                                                               scalar2=None, op0=ALU.is_gt, op1=ALU.add,
                                                               accum_out=sm[:, 3:4]),
                              reads=[Rscore, Rsm], writes=[Rjunk, Rsm])
                        K.dve(lambda e, k=k: e.tensor_scalar(out=sm[:, 4:5], in0=sm[:, 3:4], scalar1=float(TOPK),
                                                             scalar2=stepT[:, k:k + 1], op0=ALU.is_ge, op1=ALU.mult),
                              reads=[Rsm, RstepT], writes=[Rsm])
                        kk = k + 1 if k < NBIS else k
                        K.dve(lambda e, kk=kk: e.scalar_tensor_tensor(out=sm[:, 5:6], in0=sm[:, 4:5],
                                                                      scalar=stepT[:, kk:kk + 1], in1=sm[:, 5:6],
                                                                      op0=ALU.subtract, op1=ALU.add),
                              reads=[Rsm, RstepT], writes=[Rsm])
                    K.dve(lambda e, nk=nk: e.tensor_scalar(out=mask_b[:, 0:nk], in0=score[:, 0:nk], scalar1=sm[:, 5:6],
                                                           scalar2=NEGM, op0=ALU.is_le, op1=ALU.mult),
                          reads=[Rscore, Rsm], writes=[Rmask_b])
            def stA2(qt):
                mT = maskT[qt % 2]
                RmT = RmaskT[qt % 2]
                if True:
                    for k0 in range(0, qt + 1, 4):
                        nb_ = min(4, qt + 1 - k0)
                        bk = (rot["ps"] % 2)
                        rot["ps"] += 1
                        pT3 = psb[bk][:, :].bitcast(BF16)[:, 0:512].rearrange("p (c t) -> p c t", c=4)
                        for i in range(nb_):
                            K.pe(lambda e, pT3=pT3, i=i, k0=k0: e.transpose(
                                out=pT3[:, i, :], in_=mask_b[:, (k0 + i) * 128:(k0 + i + 1) * 128], identity=ident_b),
                                reads=[Rmask_b, RG["ident_b"]], writes=[RP[bk]])
                        evac_copy(mT[:, k0:k0 + nb_, :], pT3[:, 0:nb_, :], [RP[bk]], [RmT], force="dve")

            def stB(qt):
                mT = maskT[qt % 2]
                RmT = RmaskT[qt % 2]
                def fin_dsa(qt=qt):
                    for hf in range(2):
                        K.act(lambda e, hf=hf: e.copy(out=ya_f[:, hf * 4:hf * 4 + 4, :],
                                                      in_=psb[6 + hf][:, 0:260].rearrange("p (h d) -> p h d", h=4)),
                              reads=[RP[6 + hf]], writes=[Rya_f])
                    K.dve(lambda e: e.reciprocal(out=rec8, in_=ya_f[:, :, 64]), reads=[Rya_f], writes=[Rrec8])
                    K.dve(lambda e: e.tensor_tensor(out=ya_b.rearrange("p (h d) -> p h d", h=8), in0=ya_f[:, :, 0:64],
                                                    in1=_bl(rec8, 64), op=ALU.mult),
                          reads=[Rya_f, Rrec8], writes=[Rya_b])
                    bk = (rot["ps"] % 2)
                    rot["ps"] += 1
                    pT4 = psb[bk][:, :].bitcast(BF16)[:, 0:512].rearrange("p (c t) -> p c t", c=4)
                    for j in range(4):
                        K.pe(lambda e, pT4=pT4, j=j: e.transpose(out=pT4[:, j, :], in_=ya_b[:, j * 128:(j + 1) * 128],
                                                                 identity=ident_b),
                             reads=[Rya_b, RG["ident_b"]], writes=[RP[bk]])
                    evac_copy(y_aT[:, :, qt * 128:(qt + 1) * 128], pT4, [RP[bk]], [Ry_aT[qt // 4]], force="dve")

                if not NOATT:
                  attention(8, q_aT, Rq_aT, k_hT, Rk_hT, v_h, Rv_h, Vd, RVd, 15, qt,
                          mT if qt >= 2 else None, RmT, [2, 3, 4, 5], [6, 7], fin_dsa, p_sb, Rp_sb, qpads)
            for qt in range(NQT):
                if 2 <= qt + 1 < NQT:
                    stA1(qt + 1)
                stB(qt)
                if 2 <= qt + 1 < NQT:
                    stA2(qt + 1)
            barrier()

            if debug and b == 0:
                K.dma("sp", lambda e: e.dma_start(out=dbg_yaf, in_=ya_f.rearrange("p a b -> p (a b)")), reads=[Rya_f])
                K.dma("sp", lambda e: e.dma_start(out=dbg_p0, in_=p_sb[0]), reads=[Rp_sb[0]])
                K.dma("sp", lambda e: e.dma_start(out=dbg_p1, in_=p_sb[1]), reads=[Rp_sb[1]])
                K.dma("sp", lambda e: e.dma_start(out=dbg_qp, in_=qpads[0][0].rearrange("p a b -> p (a b)")), reads=[qpads[0][1]])
                K.dma("sp", lambda e: e.dma_start(out=dbg_ya.rearrange("(c p) t -> p c t", p=128), in_=y_aT), reads=Ry_aT)
            if stop_after == 'B':
                break
            R3.reset()
            acc = R3.f32(NT, 260)
            dq = R3.bf16(2, L)
            dk = R3.bf16(2, L)
            dv = R3.bf16(NT, 4, 65)
            Vg = R3.bf16(4, 2048)
            p_sbC = [R3.bf16(512) for _ in range(4)]
            WstC = [R3.bf16(8, 256) for _ in range(2)]
            VstC = [R3.bf16(2048) for _ in range(2)]
            yb_f = R3.f32(4, 65)
            rec4 = R3.f32(4)
            yb_b = R3.bf16(256)
            qpadsC = [(R3.bf16(4, 128), Res()) for _ in range(2)]
            for qp_, Rqp_ in qpadsC:
                K.pool(lambda e, qp_=qp_: e.memset(qp_, 0.0), writes=[Rqp_])
            Racc = RL(NT)
            Rdq, Rdk = RL(4), RL(4)
            Rdv = RL(NT)
            RVg = Res()
            Rp_sbC, RWstC, RVstC = RL(4), RL(2), RL(2)
            Ryb_f, Rrec4, Ryb_b = Res(), Res(), Res()
            for g in range(3):
                W_, r_ = DIL[g]
                dmax = W_ // 128
                build_V(Vg, RVg, 1 + g, VstC, RVstC, [0, 1])
                K.pool(lambda e: e.memset(dv[:, :, :, 64:65], 1.0), writes=Rdv)
                for j in range(2):
                    load_w(WstC[0], RWstC[0], C_DIL + g * 256 + j * 128, 128)
                    proj_fm(WstC[0], RWstC[0], 0, 128, uT, RuT, lambda tb, j=j: dq[:, j, tb * 512:(tb + 1) * 512], Rdq,
                            [0, 1])
                    load_w(WstC[1], RWstC[1], C_DIL + 768 + g * 256 + j * 128, 128)
                    proj_fm(WstC[1], RWstC[1], 0, 128, uT, RuT, lambda tb, j=j: dk[:, j, tb * 512:(tb + 1) * 512], Rdk,
                            [0, 1], 0.125)
                load_w(WstC[0], RWstC[0], C_DIL + 1536 + g * 256, 256)
                for tt in range(NT):
                    bk = rot["ps"] % 2
                    rot["ps"] += 1
                    for c in range(8):
                        K.pe(lambda e, bk=bk, c=c, tt=tt: e.matmul(psb[bk][:, 0:256], lhsT=uT[:, c, tt * 128:(tt + 1) * 128],
                                                                   rhs=WstC[0][:, c, 0:256], start=(c == 0), stop=(c == 7)),
                             reads=[RuT[tt], RWstC[0]], writes=[RP[bk]])
                    evac_copy(dv[:, tt, :, 0:64], psb[bk][:, 0:256].rearrange("p (h d) -> p h d", h=4), [RP[bk]],
                              [Rdv[tt]])
                for qt in range(NT):
                    def fin_dil(qt=qt, g=g):
                        if g == 0:
                            K.act(lambda e: e.copy(out=acc[:, qt, :], in_=psb[6][:, 0:260]), reads=[RP[6]],
                                  writes=[Racc[qt]])
                        elif g == 1:
                            K.dve(lambda e: e.tensor_tensor(out=acc[:, qt, :], in0=psb[6][:, 0:260], in1=acc[:, qt, :],
                                                            op=ALU.add), reads=[RP[6], Racc[qt]], writes=[Racc[qt]])
                        else:
                            K.dve(lambda e: e.tensor_tensor(out=yb_f.rearrange("p h d -> p (h d)"), in0=psb[6][:, 0:260],
                                                            in1=acc[:, qt, :], op=ALU.add),
                                  reads=[RP[6], Racc[qt]], writes=[Ryb_f])
                            K.dve(lambda e: e.reciprocal(out=rec4, in_=yb_f[:, :, 64]), reads=[Ryb_f], writes=[Rrec4])
                            K.dve(lambda e: e.tensor_tensor(out=yb_b.rearrange("p (h d) -> p h d", h=4),
                                                            in0=yb_f[:, :, 0:64], in1=_bl(rec4, 64), op=ALU.mult),
                                  reads=[Ryb_f, Rrec4], writes=[Ryb_b])
                            bk = (rot["ps"] % 2)
                            rot["ps"] += 1
                            pT5 = psb[bk][:, :].bitcast(BF16)[:, 0:256].rearrange("p (c t) -> p c t", c=2)
                            for j in range(2):
                                K.pe(lambda e, pT5=pT5, j=j: e.transpose(out=pT5[:, j, :],
                                                                         in_=yb_b[:, j * 128:(j + 1) * 128],
                                                                         identity=ident_b),
                                     reads=[Ryb_b, RG["ident_b"]], writes=[RP[bk]])
                            evac_copy(y_bT[:, :, qt * 128:(qt + 1) * 128], pT5, [RP[bk]], [Ry_bT[qt // 4]])

                    attention(4, dq, Rdq, dk, Rdk, dv, Rdv, Vg, RVg, dmax, qt, None, None, [2, 3, 4, 5], [6], fin_dil,
                              p_sbC, Rp_sbC, qpadsC)
            barrier()

            if debug and b == 0:
                K.dma("sp", lambda e: e.dma_start(out=dbg_ya.rearrange("(c p) t -> p c t", p=128), in_=y_aT), reads=Ry_aT)
                K.dma("sp", lambda e: e.dma_start(out=dbg_yb.rearrange("(c p) t -> p c t", p=128), in_=y_bT), reads=Ry_bT)
            if stop_after == 'C':
                break
            R3.reset()
            mixT = R3.bf16(8, L)
            markD = R3.p
            Wg_ = [R3.bf16(8, 256) for _ in range(2)]
            Wa = R3.bf16(4, 1024)
            Wb = R3.bf16(2, 1024)
            sig = [R3.f32(512) for _ in range(4)]
            mm_ = [R3.f32(512) for _ in range(4)]
            RmixT = RL(16)
            RWg_, RWa, RWb, Rsig, Rmm_ = RL(2), Res(), Res(), RL(4), RL(4)
            K.dma("pool", lambda e: e.dma_start(out=Wa, in_=w_branch_a[0].rearrange("(c p) n -> p c n", p=128)),
                  writes=[RWa])
            K.dma("pool", lambda e: e.dma_start(out=Wb, in_=w_branch_b[0].rearrange("(c p) n -> p c n", p=128)),
                  writes=[RWb])
            it = 0
            for oc in range(8):
                wi = oc % 2
                load_w(Wg_[wi], RWg_[wi], C_G + oc * 128, 128, 0)
                load_w(Wg_[wi], RWg_[wi], C_G + 1024 + oc * 128, 128, 128)
                for tb in range(4):
                    s0, s1 = (it * 2) % 4, (it * 2 + 1) % 4
                    rb4 = 4 * (it % 2)
                    it += 1
                    for br in range(2):
                        bk = br + rb4
                        for c in range(8):
                            K.pe(lambda e, bk=bk, c=c, br=br, wi=wi, tb=tb: e.matmul(
                                psb[bk][:, :], lhsT=Wg_[wi][:, c, br * 128:(br + 1) * 128],
                                rhs=uT[:, c, tb * 512:(tb + 1) * 512], start=(c == 0), stop=(c == 7)),
                                reads=[RWg_[wi]] + RuT[tb * 4:(tb + 1) * 4], writes=[RP[bk]])
                        si = s0 if br == 0 else s1
                        K.act(lambda e, bk=bk, si=si: e.activation(out=sig[si], in_=psb[bk][:, :], func=AF.Sigmoid),
                              reads=[RP[bk]], writes=[Rsig[si]])
                    for f in range(4):
                        K.pe(lambda e, f=f, oc=oc, tb=tb, ba=2 + rb4: e.matmul(psb[ba][:, :], lhsT=Wa[:, f, oc * 128:(oc + 1) * 128],
                                                                   rhs=y_aT[:, f, tb * 512:(tb + 1) * 512],
                                                                   start=(f == 0), stop=(f == 3)),
                             reads=[RWa, Ry_aT[tb]], writes=[RP[2 + rb4]])
                    for f in range(2):
                        K.pe(lambda e, f=f, oc=oc, tb=tb, bb=3 + rb4: e.matmul(psb[bb][:, :], lhsT=Wb[:, f, oc * 128:(oc + 1) * 128],
                                                                   rhs=y_bT[:, f, tb * 512:(tb + 1) * 512],
                                                                   start=(f == 0), stop=(f == 1)),
                             reads=[RWb, Ry_bT[tb]], writes=[RP[3 + rb4]])
                    K.dve(lambda e, s0=s0, ba=2 + rb4: e.tensor_tensor(out=mm_[s0], in0=psb[ba][:, :], in1=sig[s0], op=ALU.mult),
                          reads=[RP[2 + rb4], Rsig[s0]], writes=[Rmm_[s0]])
                    K.dve(lambda e, s1=s1, bb=3 + rb4: e.tensor_tensor(out=mm_[s1], in0=psb[bb][:, :], in1=sig[s1], op=ALU.mult),
                          reads=[RP[3 + rb4], Rsig[s1]], writes=[Rmm_[s1]])
                    K.pool(lambda e, s0=s0, s1=s1, oc=oc, tb=tb: e.tensor_tensor(
                        out=mixT[:, oc, tb * 512:(tb + 1) * 512], in0=mm_[s0], in1=mm_[s1], op=ALU.add),
                        reads=[Rmm_[s0], Rmm_[s1]], writes=RmixT[tb * 4:(tb + 1) * 4])
            barrier()

            if debug and b == 0:
                K.dma("sp", lambda e: e.dma_start(out=dbg_mix.rearrange("(c p) t -> p c t", p=128), in_=mixT), reads=RmixT)
            if stop_after == 'D':
                break
            R1.reset()
            hh = R1.f32(NT, 1024)
            Rh = RL(NT)
            R3.p = markD
            hnT = R3.bf16(8, L)
            RhnT = RL(4)
            comb = R3.f32(NT, 32)
            Rcomb = RL(NT)
            markE = R3.p
            Wo = R3.bf16(8, 1024)
            RWo = Res()
            K.dma("pool", lambda e: e.dma_start(out=Wo, in_=w_out[0].rearrange("(c p) n -> p c n", p=128)), writes=[RWo])
            hn_f = [R3.f32(1024) for _ in range(2)]
            hnT_f = [R3.f32(8, 128) for _ in range(2)]
            ssE = [R3.f32(1) for _ in range(2)]
            rsE = [R3.f32(1) for _ in range(2)]
            lgA = R3.f32(NT, 36)
            elA = R3.f32(NT, 32)
            oh1 = R3.f32(NT, 32)
            oh2 = R3.f32(NT, 32)
            ex4 = R3.f32(NT, 4)
            pen = R3.f32(NT, 4)
            gm, pg, m1, m2, dd, w1p, w2p = [R3.f32(NT) for _ in range(7)]
            Rhn_f, RhnT_f, RssE, RrsE = RL(2), RL(2), RL(2), RL(2)
            RlgA, RelA, Roh1, Roh2, Rex4, Rpen, Rrt2 = Res(), Res(), Res(), Res(), Res(), Res(), Res()
            for tt in range(NT):
                i2 = tt % 2
                K.dma("sp", lambda e, tt=tt, b=b: e.dma_start(out=hh[:, tt, :], in_=x[b, tt * 128:(tt + 1) * 128, :]),
                      writes=[Rh[tt]])
                for nh in range(2):
                    bk = nh + 6 * (tt % 2)
                    for c in range(8):
                        K.pe(lambda e, bk=bk, c=c, tt=tt, nh=nh: e.matmul(
                            psb[bk][:, :], lhsT=mixT[:, c, tt * 128:(tt + 1) * 128], rhs=Wo[:, c, nh * 512:(nh + 1) * 512],
                            start=(c == 0), stop=(c == 7)), reads=[RmixT[tt], RWo], writes=[RP[bk]])
                    K.dve(lambda e, bk=bk, tt=tt, nh=nh: e.tensor_tensor(
                        out=hh[:, tt, nh * 512:(nh + 1) * 512], in0=psb[bk][:, :], in1=hh[:, tt, nh * 512:(nh + 1) * 512],
                        op=ALU.add), reads=[RP[bk], Rh[tt]], writes=[Rh[tt]])
                K.act(lambda e, tt=tt, i2=i2: e.activation(out=hn_f[i2], in_=hh[:, tt, :], func=AF.Square,
                                                           accum_out=ssE[i2]),
                      reads=[Rh[tt]], writes=[Rhn_f[i2], RssE[i2]])
                rms_rstd(ssE[i2], rsE[i2], RssE[i2], RrsE[i2], 1024)
                K.act(lambda e, tt=tt, i2=i2: e.activation(out=hn_f[i2], in_=hh[:, tt, :], func=AF.Copy,
                                                           scale=rsE[i2][:, 0:1]),
                      reads=[Rh[tt], RrsE[i2]], writes=[Rhn_f[i2]])
                for c in range(8):
                    bk = 2 + c // 4
                    K.pe(lambda e, bk=bk, c=c, i2=i2: e.transpose(out=psb[bk][:, (c % 4) * 128:(c % 4 + 1) * 128],
                                                                  in_=hn_f[i2][:, c * 128:(c + 1) * 128],
                                                                  identity=ident_f),
                         reads=[Rhn_f[i2], RG["ident_f"]], writes=[RP[bk]])
                for hf in range(2):
                    K.dve(lambda e, hf=hf, i2=i2: e.tensor_tensor(
                        out=hnT_f[i2][:, hf * 4:hf * 4 + 4, :], in0=psb[2 + hf][:, :].rearrange("p (c t) -> p c t", c=4),
                        in1=_bl(gcol_ffn[:, hf * 4:hf * 4 + 4], 128), op=ALU.mult),
                        reads=[RP[2 + hf], RG["gcol_ffn"]], writes=[RhnT_f[i2]])
                K.pool(lambda e, tt=tt, i2=i2: e.tensor_copy(out=hnT[:, :, tt * 128:(tt + 1) * 128], in_=hnT_f[i2]),
                       reads=[RhnT_f[i2]], writes=[RhnT[tt // 4]])
                for c in range(8):
                    K.pe(lambda e, c=c, i2=i2: e.matmul(psb[4 + i2][:, 0:36], lhsT=hnT_f[i2][:, c, :], rhs=wr[:, c, :],
                                                        start=(c == 0), stop=(c == 7)),
                         reads=[RhnT_f[i2], RG["wr"]], writes=[RP[4 + i2]])
                K.dve(lambda e, i2=i2, tt=tt: e.tensor_tensor(out=lgA[:, tt, :], in0=psb[4 + i2][:, 0:36], in1=rb_b, op=ALU.add),
                      reads=[RP[4 + i2], RG["rb_b"]], writes=[RlgA])

            lg4 = lgA[:, :, 0:4]
            elv = lgA[:, :, 4:36]
            K.dve(lambda e: e.tensor_reduce(out=gm, in_=lg4, axis=AX.X, op=ALU.max), reads=[RlgA], writes=[Rrt2])
            K.dve(lambda e: e.tensor_tensor(out=ex4, in0=lg4, in1=_bl(gm, 4), op=ALU.subtract), reads=[RlgA, Rrt2], writes=[Rex4])
            K.act(lambda e: e.activation(out=ex4, in_=ex4, func=AF.Exp), reads=[Rex4], writes=[Rex4])
            K.dve(lambda e: e.tensor_reduce(out=pg, in_=ex4, axis=AX.X, op=ALU.add), reads=[Rex4], writes=[Rrt2])
            K.dve(lambda e: e.reciprocal(out=pg, in_=pg), reads=[Rrt2], writes=[Rrt2])
            K.dve(lambda e: e.tensor_tensor(out=pen, in0=lg4, in1=_bl(gm, 4), op=ALU.is_ge), reads=[RlgA, Rrt2], writes=[Rpen])
            K.dve(lambda e: e.tensor_scalar(out=pen, in0=pen, scalar1=1e9, scalar2=-1e9, op0=ALU.mult, op1=ALU.add),
                  reads=[Rpen], writes=[Rpen])
            K.dve(lambda e: e.tensor_tensor(out=elA.rearrange("p t (g e) -> p t g e", g=4),
                                            in0=elv.rearrange("p t (g e) -> p t g e", g=4), in1=_bl(pen, 8), op=ALU.add),
                  reads=[RlgA, Rpen], writes=[RelA])
            K.dve(lambda e: e.tensor_reduce(out=m1, in_=elA, axis=AX.X, op=ALU.max), reads=[RelA], writes=[Rrt2])
            K.dve(lambda e: e.tensor_tensor(out=oh1, in0=elA, in1=_bl(m1, 32), op=ALU.is_ge), reads=[RelA, Rrt2], writes=[Roh1])
            K.dve(lambda e: e.scalar_tensor_tensor(out=elA, in0=oh1, scalar=-1e9, in1=elA, op0=ALU.mult, op1=ALU.add),
                  reads=[Roh1, RelA], writes=[RelA])
            K.dve(lambda e: e.tensor_reduce(out=m2, in_=elA, axis=AX.X, op=ALU.max), reads=[RelA], writes=[Rrt2])
            K.dve(lambda e: e.tensor_tensor(out=oh2, in0=elA, in1=_bl(m2, 32), op=ALU.is_ge), reads=[RelA, Rrt2], writes=[Roh2])
            K.dve(lambda e: e.tensor_tensor(out=dd, in0=m2, in1=m1, op=ALU.subtract), reads=[Rrt2], writes=[Rrt2])
            K.act(lambda e: e.activation(out=dd, in_=dd, func=AF.Exp), reads=[Rrt2], writes=[Rrt2])
            K.dve(lambda e: e.tensor_scalar(out=dd, in0=dd, scalar1=1.0, scalar2=None, op0=ALU.add), reads=[Rrt2], writes=[Rrt2])
            K.dve(lambda e: e.reciprocal(out=w1p, in_=dd), reads=[Rrt2], writes=[Rrt2])
            K.dve(lambda e: e.tensor_tensor(out=w1p, in0=w1p, in1=pg, op=ALU.mult), reads=[Rrt2], writes=[Rrt2])
            K.dve(lambda e: e.tensor_tensor(out=w2p, in0=pg, in1=w1p, op=ALU.subtract), reads=[Rrt2], writes=[Rrt2])
            K.dve(lambda e: e.tensor_tensor(out=comb, in0=oh1, in1=_bl(w1p, 32), op=ALU.mult), reads=[Roh1, Rrt2], writes=Rcomb)
            K.dve(lambda e: e.tensor_tensor(out=oh2, in0=oh2, in1=_bl(w2p, 32), op=ALU.mult), reads=[Roh2, Rrt2], writes=[Roh2])
            K.dve(lambda e: e.tensor_tensor(out=comb, in0=comb, in1=oh2, op=ALU.add), reads=[Roh2] + Rcomb, writes=Rcomb)

            if debug and b == 0:
                K.dma("sp", lambda e: e.dma_start(out=dbg_h.rearrange("(t p) d -> p t d", p=128), in_=hh), reads=Rh)
                K.dma("sp", lambda e: e.dma_start(out=dbg_comb.rearrange("(t p) d -> p t d", p=128), in_=comb), reads=Rcomb)
                K.dma("sp", lambda e: e.dma_start(out=dbg_hn.rearrange("(c p) t -> p c t", p=128), in_=hnT), reads=RhnT)
            if stop_after == 'E':
                break
            barrier()
            FA = Arena(R3.lo, R3.lo + 8192)
            Wgu = [FA.bf16(2, 8, 512) for _ in range(2)]
            R3.p = markE
            hid = R3.bf16(4, L)
            Rhid = RL(4)
            Wd = [R3.bf16(4, 1024) for _ in range(2)]
            sg = [R3.f32(512) for _ in range(2)]
            RWgu, RWd, Rsg = RL(2), RL(2), RL(2)
            it = 0
            for ex in range(NEXP):
                wi = ex % 2
                K.dma("pool", lambda e, wi=wi, ex=ex: e.dma_start(out=Wgu[wi][:, 0, :, :],
                                                                  in_=w_gate[0, ex].rearrange("(c p) n -> p c n", p=128)),
                      writes=[RWgu[wi]])
                K.dma("pool", lambda e, wi=wi, ex=ex: e.dma_start(out=Wgu[wi][:, 1, :, :],
                                                                  in_=w_up[0, ex].rearrange("(c p) n -> p c n", p=128)),
                      writes=[RWgu[wi]])
                K.dma("pool", lambda e, wi=wi, ex=ex: e.dma_start(out=Wd[wi], in_=w_down[0, ex].rearrange("(c p) n -> p c n", p=128)),
                      writes=[RWd[wi]])
                for tb in range(4):
                    for fc in range(4):
                        bg, bu = (it % 2) * 2, (it % 2) * 2 + 1
                        si = it % 2
                        it += 1
                        for gu, bk in ((0, bg), (1, bu)):
                            for c in range(8):
                                K.pe(lambda e, gu=gu, bk=bk, c=c, wi=wi, fc=fc, tb=tb: e.matmul(
                                    psb[bk][:, :], lhsT=Wgu[wi][:, gu, c, fc * 128:(fc + 1) * 128],
                                    rhs=hnT[:, c, tb * 512:(tb + 1) * 512], start=(c == 0), stop=(c == 7)),
                                    reads=[RWgu[wi], RhnT[tb]], writes=[RP[bk]])
                        K.act(lambda e, bg=bg, si=si: e.activation(out=sg[si], in_=psb[bg][:, :], func=AF.Silu),
                              reads=[RP[bg]], writes=[Rsg[si]])
                        K.dve(lambda e, bu=bu, si=si, fc=fc, tb=tb: e.tensor_tensor(
                            out=hid[:, fc, tb * 512:(tb + 1) * 512], in0=psb[bu][:, :], in1=sg[si], op=ALU.mult),
                            reads=[RP[bu], Rsg[si]], writes=[Rhid[tb]])
                for tt in range(NT):
                    for nh in range(2):
                        bk = 4 + (tt * 2 + nh) % 4
                        for fc in range(4):
                            K.pe(lambda e, bk=bk, fc=fc, tt=tt, nh=nh, wi=wi: e.matmul(
                                psb[bk][:, :], lhsT=hid[:, fc, tt * 128:(tt + 1) * 128],
                                rhs=Wd[wi][:, fc, nh * 512:(nh + 1) * 512], start=(fc == 0), stop=(fc == 3)),
                                reads=[Rhid[tt // 4], RWd[wi]], writes=[RP[bk]])
                        K.dve(lambda e, bk=bk, tt=tt, nh=nh, ex=ex: e.scalar_tensor_tensor(
                            out=hh[:, tt, nh * 512:(nh + 1) * 512], in0=psb[bk][:, :], scalar=comb[:, tt, ex:ex + 1],
                            in1=hh[:, tt, nh * 512:(nh + 1) * 512], op0=ALU.mult, op1=ALU.add),
                            reads=[RP[bk], Rcomb[tt], Rh[tt]], writes=[Rh[tt]])

            if stop_after == 'F':
                break
            ot = [R3.f32(1024) for _ in range(2)]
            ssG = [R3.f32(1) for _ in range(2)]
            rsG = [R3.f32(1) for _ in range(2)]
            sqG = R3.bf16(1024)
            Rot, RssG, RrsG, RsqG = RL(2), RL(2), RL(2), Res()
            for tt in range(NT):
                i2 = tt % 2
                K.act(lambda e, tt=tt, i2=i2: e.activation(out=sqG, in_=hh[:, tt, :], func=AF.Square, accum_out=ssG[i2]),
                      reads=[Rh[tt]], writes=[RsqG, RssG[i2]])
                rms_rstd(ssG[i2], rsG[i2], RssG[i2], RrsG[i2], 1024)
                K.dve(lambda e, tt=tt, i2=i2: e.scalar_tensor_tensor(out=ot[i2], in0=hh[:, tt, :], scalar=rsG[i2][:, 0:1],
                                                                     in1=fin_b, op0=ALU.mult, op1=ALU.mult),
                      reads=[Rh[tt], RrsG[i2], RG["fin_b"]], writes=[Rot[i2]])
                K.dma("sp", lambda e, tt=tt, i2=i2, b=b: e.dma_start(out=y[b, tt * 128:(tt + 1) * 128, :], in_=ot[i2]),
                      reads=[Rot[i2]])
            barrier()

        K.emit(block, sems, dsems)
    return nc


_NC_CACHE = {}


def _run(inputs, NB, NEXP=32, full=False):
    ncores = 8
    if NB not in _NC_CACHE:
        _NC_CACHE[NB] = build_nc(NB)
    nc = _NC_CACHE[NB]
    x = np.asarray(inputs["x"], dtype=np.float32)
    per = x.shape[0] // ncores
    oh = _onehot_tables()
    pow2 = (2.0 ** -np.arange(32, dtype=np.float64)).astype(np.float32)[None, :]
    shared = {k: np.ascontiguousarray(np.asarray(v, dtype=np.float32)) for k, v in inputs.items() if k != "x"}
    if NEXP != 32:
        for k in ("w_gate", "w_up", "w_down"):
            shared[k] = np.ascontiguousarray(shared[k][:, :NEXP])
    shared["ohtab"] = oh
    shared["pow2"] = pow2
    in_maps = []
    for c in range(ncores):
        m = dict(shared)
        m["x"] = np.ascontiguousarray(x[c * per:c * per + NB])
        in_maps.append(m)
    res = run_bass_kernel_spmd(nc, in_maps, core_ids=list(range(ncores)))
    if full:
        return res.results
    return [r["y"] for r in res.results]


def kernel(**inputs):
    outs = _run(inputs, 4)
    return np.concatenate(outs, axis=0).astype(np.float32)
```
